# Optimizing a Trainium2 kernel written in Bass

```python
import jax, jax.numpy as jnp
from jax import lax
import numpy as np

D_MODEL = 4096
BATCH = 4
SEQ = 4096
DEPTH = 1

N_META = 16
ROPE_THETA = 500000.0
Q_BLOCK = 128
NORM_EPS = 1e-6
MLA_HEADS = 16
MLA_Q_LORA = 1024
MLA_KV_LORA = 512
MLA_NOPE = 128
MLA_ROPE = 64
MLA_V = 128
DSA_HEADS = 16
DSA_KV_HEADS = 4
DSA_HEAD_DIM = 128
DSA_ROT = DSA_HEAD_DIM // 4
IDX_HEADS = 16
IDX_DIM = 64
IDX_ROT = IDX_DIM // 4
DSA_TOPK_MAX = 256
PEER_HEADS = 8
PEER_N_KEYS = 128
PEER_N_EXPERTS = PEER_N_KEYS * PEER_N_KEYS
PEER_KEY_DIM = 256
PEER_HALF = PEER_KEY_DIM // 2
PEER_TOPK = 16
PEER_BLOCK = 64

IN_SPLITS = (
    MLA_Q_LORA,
    MLA_KV_LORA,
    MLA_ROPE,
    DSA_HEADS * DSA_HEAD_DIM,
    DSA_KV_HEADS * DSA_HEAD_DIM,
    DSA_KV_HEADS * DSA_HEAD_DIM,
    IDX_HEADS * IDX_DIM,
    IDX_DIM,
    IDX_HEADS,
    D_MODEL,
    D_MODEL,
)
IN_WIDTH = sum(IN_SPLITS)
BRANCH_A_WIDTH = MLA_HEADS * MLA_V
BRANCH_B_WIDTH = DSA_HEADS * DSA_HEAD_DIM

kernel_name = "hybrid_mla_dsa_peer_block"


def rms_norm(x, g):
    xf = x.astype(jnp.float32)
    y = xf * lax.rsqrt(jnp.mean(xf * xf, axis=-1, keepdims=True) + NORM_EPS)
    return (y * g.astype(jnp.float32)).astype(x.dtype)


def rope_tables(pos, rot_dim):
    inv = ROPE_THETA ** (-jnp.arange(0, rot_dim, 2, dtype=jnp.float32) / rot_dim)
    ang = pos.astype(jnp.float32)[:, None] * inv[None, :]
    return jnp.cos(ang), jnp.sin(ang)


def apply_rope(x, cos, sin, rot_dim):
    xr, xp = x[..., :rot_dim], x[..., rot_dim:]
    x1, x2 = jnp.split(xr.astype(jnp.float32), 2, axis=-1)
    c, s = cos[:, None, :], sin[:, None, :]
    rot = jnp.concatenate([x1 * c - x2 * s, x2 * c + x1 * s], axis=-1).astype(x.dtype)
    return jnp.concatenate([rot, xp], axis=-1)


def to_blocks(a, nb):
    return jnp.swapaxes(a.reshape((a.shape[0], nb, Q_BLOCK) + a.shape[2:]), 0, 1)


def causal_dense_attention(q, k, v, scale):
    B, T, H, _ = q.shape
    nb = T // Q_BLOCK
    kpos = jnp.arange(T)

    def one(args):
        qi, i = args
        qpos = i * Q_BLOCK + jnp.arange(Q_BLOCK)
        s = jnp.einsum('bqhd,bkhd->bhqk', qi, k, preferred_element_type=jnp.float32) * scale
        s = jnp.where((kpos[None, :] <= qpos[:, None])[None, None], s, -jnp.inf)
        p = jax.nn.softmax(s, axis=-1)
        o = jnp.einsum('bhqk,bkhd->bqhd', p, v.astype(jnp.float32))
        return o.astype(q.dtype)

    out = lax.map(one, (to_blocks(q, nb), jnp.arange(nb)))
    return jnp.swapaxes(out, 0, 1).reshape(B, T, -1)


def dsa_attention(q, k, v, q_ix, k_ix, w_ix, topk):
    B, T, H, Dh = q.shape
    G = k.shape[2]
    nb = T // Q_BLOCK
    kpos = jnp.arange(T)
    gather = jax.vmap(lambda src, idx: src[idx])

    def one(args):
        qi, qix, wix, i = args
        qpos = i * Q_BLOCK + jnp.arange(Q_BLOCK)
        dots = jnp.einsum('bqhd,bkd->bqhk', qix, k_ix,
                          preferred_element_type=jnp.float32) * (IDX_DIM ** -0.5)
        score = jnp.einsum('bqh,bqhk->bqk', wix.astype(jnp.float32) * (IDX_HEADS ** -0.5),
                           jax.nn.relu(dots))
        score = jnp.where((kpos[None, :] <= qpos[:, None])[None], score, -jnp.inf)
        _, idx = lax.top_k(score, topk)
        sel_ok = idx <= qpos[None, :, None]
        k_sel = gather(k, idx)
        v_sel = gather(v, idx)
        qg = qi.reshape(B, Q_BLOCK, G, H // G, Dh)
        s = jnp.einsum('bqghd,bqkgd->bqghk', qg, k_sel,
                       preferred_element_type=jnp.float32) * (DSA_HEAD_DIM ** -0.5)
        s = jnp.where(sel_ok[:, :, None, None, :], s, -jnp.inf)
        p = jax.nn.softmax(s, axis=-1)
        o = jnp.einsum('bqghk,bqkgd->bqghd', p, v_sel.astype(jnp.float32))
        return o.reshape(B, Q_BLOCK, H * Dh).astype(q.dtype)

    out = lax.map(one, (to_blocks(q, nb), to_blocks(q_ix, nb), to_blocks(w_ix, nb), jnp.arange(nb)))
    return jnp.swapaxes(out, 0, 1).reshape(B, T, H * Dh)


def hybrid_mixer(hn, w_in, q_norm_g, w_uq, kv_norm_g, w_ukv, w_branch, w_out,
                 rope_mla, rope_dsa, rope_idx, topk):
    B, T, _ = hn.shape
    proj = hn @ w_in
    offs = np.cumsum(IN_SPLITS)[:-1].tolist()
    (c_q, c_kv, k_pe, q_b, k_b, v_b, q_ix, k_ix, w_ix, gate_a, gate_b) = jnp.split(proj, offs, axis=-1)

    q_a = (rms_norm(c_q, q_norm_g) @ w_uq).reshape(B, T, MLA_HEADS, MLA_NOPE + MLA_ROPE)
    q_a = jnp.concatenate([q_a[..., :MLA_NOPE],
                           apply_rope(q_a[..., MLA_NOPE:], *rope_mla, MLA_ROPE)], axis=-1)
    kv = (rms_norm(c_kv, kv_norm_g) @ w_ukv).reshape(B, T, MLA_HEADS, MLA_NOPE + MLA_V)
    k_pe = apply_rope(k_pe[:, :, None, :], *rope_mla, MLA_ROPE)
    k_a = jnp.concatenate([kv[..., :MLA_NOPE],
                           jnp.broadcast_to(k_pe, (B, T, MLA_HEADS, MLA_ROPE))], axis=-1)
    v_a = kv[..., MLA_NOPE:]
    o_a = causal_dense_attention(q_a, k_a, v_a, (MLA_NOPE + MLA_ROPE) ** -0.5)

    q_b = apply_rope(q_b.reshape(B, T, DSA_HEADS, DSA_HEAD_DIM), *rope_dsa, DSA_ROT)
    k_b = apply_rope(k_b.reshape(B, T, DSA_KV_HEADS, DSA_HEAD_DIM), *rope_dsa, DSA_ROT)
    v_b = v_b.reshape(B, T, DSA_KV_HEADS, DSA_HEAD_DIM)
    q_ix = apply_rope(q_ix.reshape(B, T, IDX_HEADS, IDX_DIM), *rope_idx, IDX_ROT)
    k_ix = apply_rope(k_ix[:, :, None, :], *rope_idx, IDX_ROT)[:, :, 0, :]
    o_b = dsa_attention(q_b, k_b, v_b, q_ix, k_ix, w_ix, topk)

    y_a = o_a @ w_branch[:BRANCH_A_WIDTH]
    y_b = o_b @ w_branch[BRANCH_A_WIDTH:]
    merged = jax.nn.sigmoid(gate_a) * y_a + jax.nn.sigmoid(gate_b) * y_b
    return merged @ w_out


def peer_ffn(hn, w_q, sub_keys, u_tab, v_tab):
    B, T, D = hn.shape
    q = (hn @ w_q).reshape(B, T, PEER_HEADS, 2, PEER_HALF)
    s = jnp.einsum('bthcd,hcnd->bthcn', q, sub_keys, preferred_element_type=jnp.float32)
    s1, i1 = lax.top_k(s[..., 0, :], PEER_TOPK)
    s2, i2 = lax.top_k(s[..., 1, :], PEER_TOPK)
    cand_s = (s1[..., :, None] + s2[..., None, :]).reshape(B, T, PEER_HEADS, PEER_TOPK * PEER_TOPK)
    cand_i = (i1[..., :, None] * PEER_N_KEYS + i2[..., None, :]).reshape(B, T, PEER_HEADS, PEER_TOPK * PEER_TOPK)
    top_s, top_pos = lax.top_k(cand_s, PEER_TOPK)
    experts = jnp.take_along_axis(cand_i, top_pos, axis=-1)
    gates = jax.nn.softmax(top_s, axis=-1)

    n_sel = PEER_HEADS * PEER_TOPK
    nblk = (B * T) // PEER_BLOCK
    hb = hn.reshape(nblk, PEER_BLOCK, D)
    eb = experts.reshape(nblk, PEER_BLOCK, n_sel)
    gb = gates.reshape(nblk, PEER_BLOCK, n_sel)

    def one(args):
        h_blk, e_blk, g_blk = args
        u = u_tab[e_blk]
        a = jnp.einsum('pd,ped->pe', h_blk, u, preferred_element_type=jnp.float32)
        a = jax.nn.gelu(a) * g_blk
        v = v_tab[e_blk]
        o = jnp.einsum('pe,ped->pd', a.astype(v.dtype), v, preferred_element_type=jnp.float32)
        return o.astype(h_blk.dtype)

    return lax.map(one, (hb, eb, gb)).reshape(B, T, D)


def setup_inputs(seed: int = 0) -> dict:
    key = jax.random.key(seed)
    ks = jax.random.split(key, 16)
    f32 = jnp.float32

    def nrm(k, shape, fan_in):
        return jax.random.normal(k, shape, f32) * (fan_in ** -0.5)

    def gain(k, shape):
        return 1.0 + 0.05 * jax.random.normal(k, shape, f32)

    return {
        "x": jax.random.normal(ks[0], (BATCH, SEQ, D_MODEL), f32),
        "meta_tokens": jax.random.normal(ks[1], (N_META, D_MODEL), f32),
        "attn_norm_g": gain(ks[2], (DEPTH, D_MODEL)),
        "w_in": nrm(ks[3], (DEPTH, D_MODEL, IN_WIDTH), D_MODEL),
        "q_norm_g": gain(ks[4], (DEPTH, MLA_Q_LORA)),
        "w_uq": nrm(ks[5], (DEPTH, MLA_Q_LORA, MLA_HEADS * (MLA_NOPE + MLA_ROPE)), MLA_Q_LORA),
        "kv_norm_g": gain(ks[6], (DEPTH, MLA_KV_LORA)),
        "w_ukv": nrm(ks[7], (DEPTH, MLA_KV_LORA, MLA_HEADS * (MLA_NOPE + MLA_V)), MLA_KV_LORA),
        "w_branch": nrm(ks[8], (DEPTH, BRANCH_A_WIDTH + BRANCH_B_WIDTH, D_MODEL), BRANCH_A_WIDTH),
        "w_out": nrm(ks[9], (DEPTH, D_MODEL, D_MODEL), D_MODEL),
        "ffn_norm_g": gain(ks[10], (DEPTH, D_MODEL)),
        "peer_w_q": nrm(ks[11], (DEPTH, D_MODEL, PEER_HEADS * PEER_KEY_DIM), D_MODEL),
        "peer_sub_keys": nrm(ks[12], (DEPTH, PEER_HEADS, 2, PEER_N_KEYS, PEER_HALF), PEER_HALF),
        "peer_u": nrm(ks[13], (DEPTH, PEER_N_EXPERTS, D_MODEL), D_MODEL),
        "peer_v": nrm(ks[14], (DEPTH, PEER_N_EXPERTS, D_MODEL), PEER_HEADS * PEER_TOPK),
        "final_norm_g": gain(ks[15], (D_MODEL,)),
    }


def reference(x, meta_tokens, attn_norm_g, w_in, q_norm_g, w_uq, kv_norm_g, w_ukv,
              w_branch, w_out, ffn_norm_g, peer_w_q, peer_sub_keys, peer_u, peer_v,
              final_norm_g):
    B, S, D = x.shape
    T = S + N_META
    T_pad = -(-T // Q_BLOCK) * Q_BLOCK
    topk = min(DSA_TOPK_MAX, S // 4)

    meta = jnp.broadcast_to(meta_tokens.astype(x.dtype)[None], (B, N_META, D))
    h = jnp.concatenate([meta, x, jnp.zeros((B, T_pad - T, D), x.dtype)], axis=1)

    pos = jnp.arange(T_pad)
    rope_mla = rope_tables(pos, MLA_ROPE)
    rope_dsa = rope_tables(pos, DSA_ROT)
    rope_idx = rope_tables(pos, IDX_ROT)

    for l in range(DEPTH):
        h = h + hybrid_mixer(rms_norm(h, attn_norm_g[l]), w_in[l], q_norm_g[l], w_uq[l],
                             kv_norm_g[l], w_ukv[l], w_branch[l], w_out[l],
                             rope_mla, rope_dsa, rope_idx, topk)
        h = h + peer_ffn(rms_norm(h, ffn_norm_g[l]), peer_w_q[l], peer_sub_keys[l],
                         peer_u[l], peer_v[l])

    h = rms_norm(h, final_norm_g)
    return h[:, N_META:N_META + S]
```

```python
import os
import types
import numpy as np
import ml_dtypes
import concourse.bass as bass
import concourse.mybir as mybir
from concourse.bass_utils import run_bass_kernel_spmd
from contextlib import ExitStack

F32 = mybir.dt.float32
BF16 = mybir.dt.bfloat16
U32 = mybir.dt.uint32
AF = mybir.ActivationFunctionType
ALU = mybir.AluOpType
AX = mybir.AxisListType

D = 4096
NMETA = 16
SEQ = 4096
TK = 4224
NKT = 33
NQT = 17
TQ = NQT * 128
EPS = 1e-6
NEG = -1.0e30
THETA = 500000.0
NCORES = 8


class Tr:
    __slots__ = ("name", "w", "r", "sem")

    def __init__(self, name):
        self.name = name
        self.w = []
        self.r = []
        self.sem = None


class Op:
    __slots__ = ("eng", "fn", "deps", "signal", "is_dma", "semkey", "val", "epoch")


def _freeze(fn):
    if fn is None or fn.__closure__ is None:
        return fn
    cells = []
    for c in fn.__closure__:
        try:
            cells.append(types.CellType(c.cell_contents))
        except ValueError:
            cells.append(c)
    return types.FunctionType(fn.__code__, fn.__globals__, fn.__name__, fn.__defaults__, tuple(cells))


class Prog:
    ENG = ("pe", "act", "dve", "pool", "sp")

    def __init__(self, nc, es):
        self.nc = nc
        self.es = es
        self.ops = []
        self.epoch = 0
        self.h = {"pe": nc.tensor, "act": nc.scalar, "dve": nc.vector, "pool": nc.gpsimd, "sp": nc.sync}
        self.semobj = {}
        for e in self.ENG:
            self.semobj[e] = es.enter_context(nc.semaphore("sem_" + e))
        self.ndsem = 0
        self.last = {}
        self.free_sems = {}
        self.epoch_trs = []

    def tr(self, name):
        return Tr(name)

    def _dsem(self, t, eng):
        if t.sem is None:
            fl = self.free_sems.setdefault(eng, [])
            if fl:
                key = fl.pop()
            else:
                key = "d%s%d" % (eng, self.ndsem)
                self.ndsem += 1
                self.semobj[key] = self.es.enter_context(self.nc.semaphore("sem_" + key))
            t.sem = key
            self.epoch_trs.append((t, eng))
        return t.sem

    def op(self, eng, fn, reads=(), writes=(), dma=None):
        o = Op()
        o.eng = eng
        o.fn = _freeze(fn)
        o.signal = False
        o.is_dma = dma is not None
        o.semkey = self._dsem(dma, eng) if dma is not None else eng
        o.val = None
        o.epoch = self.epoch
        raw = []
        for t in reads:
            raw.extend(t.w)
        oth = []
        for t in writes:
            oth.extend(t.w)
            oth.extend(t.r)
        deps = {}
        for d, is_raw in [(x, True) for x in raw] + [(x, False) for x in oth]:
            if d.epoch != self.epoch:
                continue
            if not (o.is_dma or d.is_dma) and d.eng == eng:
                if eng == "pe":
                    continue
            deps[id(d)] = d
        o.deps = list(deps.values())
        for d in o.deps:
            d.signal = True
        for t in writes:
            t.w = [o]
            t.r = []
        for t in reads:
            if o.is_dma:
                t.r = [x for x in t.r if x.epoch == self.epoch] + [o]
            else:
                t.r = [x for x in t.r if x.epoch == self.epoch and (x.is_dma or x.eng != eng)] + [o]
        self.ops.append(o)
        self.last[o.semkey] = o
        return o

    def barrier(self):
        lasts = [o for o in self.last.values() if o.epoch == self.epoch]
        for o in lasts:
            o.signal = True
        for e in self.ENG:
            b = Op()
            b.eng = e
            b.fn = None
            b.signal = False
            b.is_dma = False
            b.semkey = e
            b.val = None
            b.epoch = self.epoch
            b.deps = [o for o in lasts if o.is_dma or o.eng != e]
            self.ops.append(b)
        self.epoch += 1
        self.last = {}
        for (t, eng) in self.epoch_trs:
            self.free_sems[eng].append(t.sem)
            t.sem = None
        self.epoch_trs = []

    def emit(self):
        tot = {k: 0 for k in self.semobj}
        known = {e: {} for e in self.ENG}
        nw = 0
        for o in self.ops:
            E = self.h[o.eng]
            need = {}
            for d in o.deps:
                if need.get(d.semkey, 0) < d.val:
                    need[d.semkey] = d.val
            kn = known[o.eng]
            for key, v in need.items():
                if kn.get(key, 0) < v:
                    E.wait_ge(self.semobj[key], v)
                    kn[key] = v
                    nw += 1
            if o.fn is None:
                continue
            ins = o.fn()
            if o.is_dma:
                tot[o.semkey] += 16
                o.val = tot[o.semkey]
                ins.then_inc(self.semobj[o.semkey], 16)
            elif o.signal:
                tot[o.semkey] += 1
                o.val = tot[o.semkey]
                ins.then_inc(self.semobj[o.semkey], 1)
        return nw


class Ring:
    uid = 0

    def __init__(self, P, es, name, shape, dtype, n, psum=False):
        self.t = []
        self.i = 0
        for k in range(n):
            Ring.uid += 1
            nm = "%s%d_%d" % (name, k, Ring.uid)
            if psum:
                tt = es.enter_context(P.nc.psum_tensor(nm, shape, dtype))
            else:
                tt = es.enter_context(P.nc.sbuf_tensor(nm, shape, dtype))
            self.t.append((tt, P.tr(nm)))

    def next(self):
        x = self.t[self.i % len(self.t)]
        self.i += 1
        return x


def groups_of(T, g=512):
    out = []
    t0 = 0
    while t0 < T:
        n = min(g, T - t0)
        out.append((t0, n))
        t0 += n
    return out


class Builder:
    def __init__(self, nphase=99, debug=()):
        self.nphase = nphase
        self.debug = set(debug)
        self.lim = int(os.environ.get("MK_LIM", "0"))
        self.nc = bass.Bass("TRN2", target_bir_lowering=False)
        self.dram = {}

    def un(self, name):
        Ring.uid += 1
        return "%s_%d" % (name, Ring.uid)

    def din(self, name, shape, dtype=F32):
        t = self.nc.dram_tensor(name, list(shape), dtype, kind="ExternalInput")
        self.dram[name] = t
        return t

    def dscr(self, name, shape, dtype=BF16):
        kind = "ExternalOutput" if name in self.debug else "Internal"
        t = self.nc.dram_tensor(name, list(shape), dtype, kind=kind)
        self.dram[name] = t
        return t

    def phase_rownorm_T(self, P, ps, src_list, g_ap, extra=None):
        nc = self.nc
        with ExitStack() as es:
            xin = Ring(P, es, "rn_x", [128, D], F32, 2)
            xad = Ring(P, es, "rn_a", [128, D], F32, 2) if extra else None
            hnr = Ring(P, es, "rn_hn", [128, D], BF16, 2)
            hTr = Ring(P, es, "rn_hT", [128, 32, 128], BF16, 2)
            junk = es.enter_context(nc.sbuf_tensor(self.un("rn_junk"), [128, D], BF16))
            junk_t = P.tr("junk")
            gbc = es.enter_context(nc.sbuf_tensor(self.un("rn_g"), [128, D], F32))
            gbc_t = P.tr("gbc")
            st = Ring(P, es, "rn_st", [128, 4], F32, 4)
            P.op("sp", lambda: nc.sync.dma_start(out=gbc[:], in_=g_ap.partition_broadcast(128)),
                 writes=[gbc_t], dma=gbc_t)
            for (src, dstT, R) in src_list:
                for i in range(min(R // 128, self.lim) if self.lim else R // 128):
                    x, xt = xin.next()
                    P.op("sp", lambda x=x, i=i, src=src: nc.sync.dma_start(out=x[:], in_=src.ap()[i * 128:(i + 1) * 128, :]),
                         writes=[xt], dma=xt)
                    if extra is not None:
                        a, at = xad.next()
                        P.op("sp", lambda a=a, i=i: nc.sync.dma_start(out=a[:], in_=extra[0].ap()[i * 128:(i + 1) * 128, :]),
                             writes=[at], dma=at)
                        P.op("dve", lambda x=x, a=a: nc.vector.tensor_tensor(out=x[:], in0=x[:], in1=a[:], op=ALU.add),
                             reads=[at, xt], writes=[xt])
                        P.op("sp", lambda x=x, i=i: nc.sync.dma_start(out=extra[1].ap()[i * 128:(i + 1) * 128, :], in_=x[:]),
                             reads=[xt], dma=xt)
                    s, s_t = st.next()
                    P.op("act", lambda x=x, s=s: nc.scalar.activation(out=junk[:], in_=x[:], func=AF.Square, accum_out=s[:, 0:1]),
                         reads=[xt], writes=[junk_t, s_t])
                    P.op("act", lambda s=s: nc.scalar.activation(out=s[:, 1:2], in_=s[:, 0:1], func=AF.Sqrt, scale=1.0 / D, bias=self.eps_ap),
                         reads=[s_t, self.const_t], writes=[s_t])
                    P.op("dve", lambda s=s: nc.vector.reciprocal(out=s[:, 2:3], in_=s[:, 1:2]), reads=[s_t], writes=[s_t])
                    hn, hnt = hnr.next()
                    P.op("dve", lambda x=x, s=s, hn=hn: nc.vector.scalar_tensor_tensor(
                        out=hn[:], in0=x[:], scalar=s[:, 2:3], op0=ALU.mult, in1=gbc[:], op1=ALU.mult),
                        reads=[xt, s_t, gbc_t], writes=[hnt])
                    hT, hTt = hTr.next()
                    for cb in range(8):
                        pt, ptt = ps.next()
                        for c in range(4):
                            cc = cb * 4 + c
                            P.op("pe", lambda pt=pt, hn=hn, c=c, cc=cc: nc.tensor.matmul(
                                pt[:, c * 128:(c + 1) * 128], lhsT=hn[:, cc * 128:(cc + 1) * 128], rhs=self.ident[:],
                                start=True, stop=True), reads=[hnt, self.const_t], writes=[ptt])
                        dst = hT[:, cb * 4:(cb + 1) * 4, :]
                        srcp = pt[:, :].rearrange("p (c t) -> p c t", c=4)
                        if cb % 2 == 0:
                            P.op("act", lambda dst=dst, srcp=srcp: nc.scalar.copy(out=dst, in_=srcp), reads=[ptt], writes=[hTt])
                        else:
                            P.op("dve", lambda dst=dst, srcp=srcp: nc.vector.tensor_copy(out=dst, in_=srcp), reads=[ptt], writes=[hTt])
                    P.op("sp", lambda hT=hT, i=i, dstT=dstT: nc.sync.dma_start(
                        out=dstT.ap()[:, :, i * 128:(i + 1) * 128].rearrange("k p t -> p k t"), in_=hT[:]),
                        reads=[hTt], dma=hTt)
        P.barrier()

    def phase_gemm(self, P, ps, Wd, Kdim, XT, T, units, rope=None, gates=None):
        nc = self.nc
        KC = Kdim // 128
        blocks = []
        cur = []
        c_start = None
        for u in units:
            w = u["m"] + u.get("mb", 0)
            if cur and (u.get("brk") or u["c0"] + w - c_start > 512 or u["kind"] == "tok" or cur[-1]["kind"] == "tok" or (u["kind"] == "branch" and u["c0"] + w - c_start > 512)
                        or u["c0"] != cur[-1]["c0"] + cur[-1]["m"] + cur[-1].get("mb", 0)):
                blocks.append((c_start, cur))
                cur = []
            if not cur:
                c_start = u["c0"]
            cur.append(u)
        if cur:
            blocks.append((c_start, cur))
        tg = groups_of(T)
        if self.lim:
            tg = tg[:max(1, self.lim // 4)]
        with ExitStack() as es:
            wr = Ring(P, es, "g_w", [128, KC, 512], BF16, 2)
            xr = Ring(P, es, "g_x", [128, KC, 512], BF16, 2)
            sr = Ring(P, es, "g_s", [128, 512], F32, 4)
            tr_ = Ring(P, es, "g_t", [128, 512], F32, 3)
            rr = Ring(P, es, "g_r", [128, 6, 512], F32, 2) if rope is not None else None
            gr = Ring(P, es, "g_g", [128, 512], BF16, 4) if gates is not None else None
            for (c_start, ulist) in blocks:
                last = ulist[-1]
                bw = last["c0"] + last["m"] + last.get("mb", 0) - c_start
                w, wt = wr.next()
                half = KC // 2 if KC >= 2 else KC
                for k0 in range(0, KC, half):
                    P.op("pool", lambda w=w, k0=k0, c_start=c_start, bw=bw, half=half: nc.gpsimd.dma_start(
                        out=w[:, k0:k0 + half, :bw],
                        in_=Wd.ap()[k0 * 128:(k0 + half) * 128, c_start:c_start + bw].rearrange("(k p) c -> p k c", p=128)),
                        writes=[wt], dma=wt)
                for (t0, n) in tg:
                    x, xt = xr.next()
                    P.op("sp", lambda x=x, t0=t0, n=n: nc.sync.dma_start(
                        out=x[:, :, :n], in_=XT.ap()[:, :, t0:t0 + n].rearrange("k p t -> p k t")),
                        writes=[xt], dma=xt)
                    if rope is not None and any(u["kind"] == "rope" for u in ulist):
                        rt, rtt = rr.next()
                        P.op("sp", lambda rt=rt, t0=t0, n=n: nc.sync.dma_start(
                            out=rt[:, :, :n], in_=rope.ap()[:, :, t0:t0 + n].rearrange("k p t -> p k t")),
                            writes=[rtt], dma=rtt)
                    for u in ulist:
                        kind = u["kind"]
                        off = u["c0"] - c_start
                        m = u["m"]
                        if kind == "tok":
                            for tt in range(n // 128):
                                pt, ptt = ps.next()
                                for kc in range(KC):
                                    P.op("pe", lambda pt=pt, x=x, w=w, kc=kc, tt=tt, off=off, m=m: nc.tensor.matmul(
                                        pt[:, :m], lhsT=x[:, kc, tt * 128:(tt + 1) * 128], rhs=w[:, kc, off:off + m],
                                        start=(kc == 0), stop=(kc == KC - 1)), reads=[xt, wt], writes=[ptt])
                                s, s_t = sr.next()
                                sv = s[:, :m] if u["dtype"] == F32 else s[:, :].bitcast(BF16)[:, :m]
                                r0 = t0 + tt * 128
                                if u.get("res") is not None:
                                    rs_, rst = tr_.next()
                                    P.op("sp", lambda rs_=rs_, u=u, r0=r0, m=m: nc.sync.dma_start(
                                        out=rs_[:, :m], in_=u["res"].ap()[r0:r0 + 128, u["col"]:u["col"] + m]), writes=[rst], dma=rst)
                                    P.op("dve", lambda sv=sv, pt=pt, rs_=rs_, m=m: nc.vector.tensor_tensor(out=sv, in0=pt[:, :m], in1=rs_[:, :m], op=ALU.add),
                                         reads=[ptt, rst], writes=[s_t])
                                else:
                                    P.op("act", lambda sv=sv, pt=pt, m=m: nc.scalar.copy(out=sv, in_=pt[:, :m]), reads=[ptt], writes=[s_t])
                                P.op("sp", lambda sv=sv, u=u, r0=r0, m=m: nc.sync.dma_start(
                                    out=u["dst"].ap()[r0:r0 + 128, u["col"]:u["col"] + m], in_=sv), reads=[s_t], dma=s_t)
                            continue
                        if kind == "branch":
                            pa_, pat = ps.next()
                            pb_, pbt = ps.next()
                            hk = KC // 2
                            for kc in range(KC):
                                tgt, tgtt = (pa_, pat) if kc < hk else (pb_, pbt)
                                P.op("pe", lambda tgt=tgt, x=x, w=w, kc=kc, off=off, n=n, hk=hk: nc.tensor.matmul(
                                    tgt[:, :n], lhsT=w[:, kc, off:off + 128], rhs=x[:, kc, :n],
                                    start=(kc % hk == 0), stop=(kc % hk == hk - 1)), reads=[xt, wt], writes=[tgtt])
                            g1, g1t = gr.next()
                            g2, g2t = gr.next()
                            P.op("sp", lambda g1=g1, u=u, t0=t0, n=n: nc.sync.dma_start(out=g1[:, :n], in_=gates[0].ap()[u["idx"], :, t0:t0 + n]),
                                 writes=[g1t], dma=g1t)
                            P.op("sp", lambda g2=g2, u=u, t0=t0, n=n: nc.sync.dma_start(out=g2[:, :n], in_=gates[1].ap()[u["idx"], :, t0:t0 + n]),
                                 writes=[g2t], dma=g2t)
                            t1, t1t = tr_.next()
                            s, s_t = sr.next()
                            sb = s[:, :].bitcast(BF16)
                            P.op("dve", lambda t1=t1, pa_=pa_, g1=g1, n=n: nc.vector.tensor_tensor(out=t1[:, :n], in0=pa_[:, :n], in1=g1[:, :n], op=ALU.mult),
                                 reads=[pat, g1t], writes=[t1t])
                            t2, t2t = tr_.next()
                            P.op("dve", lambda t2=t2, pb_=pb_, g2=g2, n=n: nc.vector.tensor_tensor(out=t2[:, :n], in0=pb_[:, :n], in1=g2[:, :n], op=ALU.mult),
                                 reads=[pbt, g2t], writes=[t2t])
                            P.op("dve", lambda sb=sb, t1=t1, t2=t2, n=n: nc.vector.tensor_tensor(out=sb[:, :n], in0=t1[:, :n], in1=t2[:, :n], op=ALU.add),
                                 reads=[t1t, t2t], writes=[s_t])
                            P.op("sp", lambda sb=sb, u=u, t0=t0, n=n: nc.sync.dma_start(
                                out=u["dst"].ap()[u["idx"], :, t0:t0 + n], in_=sb[:, :n]), reads=[s_t], dma=s_t)
                            continue
                        pt, ptt = ps.next()
                        for kc in range(KC):
                            P.op("pe", lambda pt=pt, x=x, w=w, kc=kc, off=off, m=m, n=n: nc.tensor.matmul(
                                pt[:m, :n], lhsT=w[:, kc, off:off + m], rhs=x[:, kc, :n],
                                start=(kc == 0), stop=(kc == KC - 1)), reads=[xt, wt], writes=[ptt])
                        s, s_t = sr.next()
                        sb = s[:, :].bitcast(BF16)
                        if kind == "plain":
                            P.op("act", lambda sb=sb, pt=pt, m=m, n=n: nc.scalar.copy(out=sb[:m, :n], in_=pt[:m, :n]), reads=[ptt], writes=[s_t])
                        elif kind == "sig":
                            P.op("act", lambda sb=sb, pt=pt, m=m, n=n: nc.scalar.activation(out=sb[:m, :n], in_=pt[:m, :n], func=AF.Sigmoid),
                                 reads=[ptt], writes=[s_t])
                        elif kind == "rope":
                            mb = u["mb"]
                            pb, pbt = ps.next()
                            for kc in range(KC):
                                P.op("pe", lambda pb=pb, x=x, w=w, kc=kc, off=off, m=m, mb=mb, n=n: nc.tensor.matmul(
                                    pb[:mb, :n], lhsT=w[:, kc, off + m:off + m + mb], rhs=x[:, kc, :n],
                                    start=(kc == 0), stop=(kc == KC - 1)), reads=[xt, wt], writes=[pbt])
                            P.op("act", lambda sb=sb, pt=pt, m=m, n=n: nc.scalar.copy(out=sb[:m, :n], in_=pt[:m, :n]), reads=[ptt], writes=[s_t])
                            ti = u["ti"]
                            for (r0, rc) in (u["rows"] if not os.environ.get("MK_NOROPE") else []):
                                t1, t1t = tr_.next()
                                t2, t2t = tr_.next()
                                P.op("act", lambda t1=t1, pt=pt, r0=r0, rc=rc, n=n: nc.scalar.copy(out=t1[r0:r0 + rc, :n], in_=pt[r0:r0 + rc, :n]),
                                     reads=[ptt], writes=[t1t])
                                P.op("act", lambda t2=t2, pb=pb, r0=r0, rc=rc, n=n: nc.scalar.copy(out=t2[r0:r0 + rc, :n], in_=pb[r0:r0 + rc, :n]),
                                     reads=[pbt], writes=[t2t])
                                P.op("dve", lambda t1=t1, rt=rt, r0=r0, rc=rc, n=n, ti=ti: nc.vector.tensor_tensor(
                                    out=t1[r0:r0 + rc, :n], in0=t1[r0:r0 + rc, :n], in1=rt[r0:r0 + rc, 2 * ti, :n], op=ALU.mult),
                                    reads=[t1t, rtt], writes=[t1t])
                                P.op("dve", lambda t2=t2, rt=rt, r0=r0, rc=rc, n=n, ti=ti: nc.vector.tensor_tensor(
                                    out=t2[r0:r0 + rc, :n], in0=t2[r0:r0 + rc, :n], in1=rt[r0:r0 + rc, 2 * ti + 1, :n], op=ALU.mult),
                                    reads=[t2t, rtt], writes=[t2t])
                                P.op("dve", lambda sb=sb, t1=t1, t2=t2, r0=r0, rc=rc, n=n: nc.vector.tensor_tensor(
                                    out=sb[r0:r0 + rc, :n], in0=t1[r0:r0 + rc, :n], in1=t2[r0:r0 + rc, :n], op=ALU.add),
                                    reads=[t1t, t2t], writes=[s_t])
                        P.op("sp", lambda sb=sb, u=u, t0=t0, m=m, n=n: nc.sync.dma_start(
                            out=u["dst"].ap()[u["idx"], :m, t0:t0 + n], in_=sb[:m, :n]), reads=[s_t], dma=s_t)
        P.barrier()

    def phase_fnorm(self, P, ps, jobs):
        nc = self.nc
        with ExitStack() as es:
            xr = Ring(P, es, "fn_x", [128, 8, 512], BF16, 2)
            qr = Ring(P, es, "fn_q", [128, 8, 512], BF16, 2)
            orr = Ring(P, es, "fn_o", [128, 8, 512], BF16, 2)
            rs = Ring(P, es, "fn_r", [128, 2, 512], F32, 2)
            for (src, dst, nsub, T, gcol) in jobs:
                nf = nsub * 128
                for (t0, n) in (groups_of(T)[:max(1, self.lim // 4)] if self.lim else groups_of(T)):
                    x, xt = xr.next()
                    P.op("sp", lambda x=x, src=src, nsub=nsub, t0=t0, n=n: nc.sync.dma_start(
                        out=x[:, :nsub, :n], in_=src.ap()[:, :, t0:t0 + n].rearrange("k p t -> p k t")), writes=[xt], dma=xt)
                    q, qt = qr.next()
                    P.op("act", lambda q=q, x=x, nsub=nsub, n=n: nc.scalar.activation(out=q[:, :nsub, :n], in_=x[:, :nsub, :n], func=AF.Square),
                         reads=[xt], writes=[qt])
                    pt, ptt = ps.next()
                    for i in range(nsub):
                        P.op("pe", lambda pt=pt, q=q, i=i, n=n: nc.tensor.matmul(pt[:, :n], lhsT=self.ones[:], rhs=q[:, i, :n],
                                                                                 start=(i == 0), stop=(i == nsub - 1)),
                             reads=[qt, self.const_t], writes=[ptt])
                    r, rt = rs.next()
                    P.op("act", lambda r=r, pt=pt, n=n, nf=nf: nc.scalar.activation(out=r[:, 0, :n], in_=pt[:, :n], func=AF.Sqrt, scale=1.0 / nf, bias=self.eps_ap),
                         reads=[ptt, self.const_t], writes=[rt])
                    P.op("dve", lambda r=r, n=n: nc.vector.reciprocal(out=r[:, 1, :n], in_=r[:, 0, :n]), reads=[rt], writes=[rt])
                    o, ot = orr.next()
                    for i in range(nsub):
                        P.op("dve", lambda o=o, x=x, r=r, i=i, n=n, gcol=gcol: nc.vector.scalar_tensor_tensor(
                            out=o[:, i, :n], in0=x[:, i, :n], scalar=gcol[:, i:i + 1], op0=ALU.mult, in1=r[:, 1, :n], op1=ALU.mult),
                            reads=[xt, rt, self.const_t], writes=[ot])
                    P.op("sp", lambda o=o, dst=dst, nsub=nsub, t0=t0, n=n: nc.sync.dma_start(
                        out=dst.ap()[:, :, t0:t0 + n].rearrange("k p t -> p k t"), in_=o[:, :nsub, :n]), reads=[ot], dma=ot)
        P.barrier()

    def softmax_pv(self, P, ps, R, S, St, nk, vfn, out_ap, out_t):
        nc = self.nc
        ncol = nk * 128
        s, s_t = R["st"].next()
        P.op("dve", lambda: nc.vector.tensor_reduce(out=s[:, 0:1], in_=S[:, :ncol], op=ALU.max, axis=AX.X, negate=True),
             reads=[St], writes=[s_t])
        pb, pbt = R["pb"].next()
        P.op("act", lambda: nc.scalar.activation(out=pb[:, :ncol], in_=S[:, :ncol], func=AF.Exp, bias=s[:, 0:1], scale=1.0,
                                                 accum_out=s[:, 1:2]), reads=[St, s_t], writes=[pbt, s_t])
        P.op("dve", lambda: nc.vector.reciprocal(out=s[:, 2:3], in_=s[:, 1:2]), reads=[s_t], writes=[s_t])
        dg, dgt = R["dg"].next()
        P.op("dve", lambda: nc.vector.tensor_scalar(out=dg[:], in0=self.ident[:], scalar1=s[:, 2:3], scalar2=None, op0=ALU.mult),
             reads=[s_t, self.const_t], writes=[dgt])
        PT, PTt = R["pt"].next()
        for kq in range((nk + 3) // 4):
            cnt = min(4, nk - 4 * kq)
            pt, ptt = ps.next()
            for j in range(cnt):
                kt = 4 * kq + j
                P.op("pe", lambda pt=pt, j=j, kt=kt: nc.tensor.matmul(pt[:, j * 128:(j + 1) * 128], lhsT=pb[:, kt * 128:(kt + 1) * 128],
                                                                      rhs=dg[:], start=True, stop=True), reads=[pbt, dgt], writes=[ptt])
            dst = PT[:, 4 * kq:4 * kq + cnt, :]
            src = pt[:, :cnt * 128].rearrange("p (c t) -> p c t", c=cnt)
            if kq % 2 == 0:
                P.op("act", lambda dst=dst, src=src: nc.scalar.copy(out=dst, in_=src), reads=[ptt], writes=[PTt])
            else:
                P.op("dve", lambda dst=dst, src=src: nc.vector.tensor_copy(out=dst, in_=src), reads=[ptt], writes=[PTt])
        po, pot = ps.next()
        for kt in range(nk):
            v_ap, v_t = vfn(kt)
            P.op("pe", lambda kt=kt, v_ap=v_ap: nc.tensor.matmul(po[:, :128], lhsT=v_ap, rhs=PT[:, kt, :], start=(kt == 0), stop=(kt == nk - 1)),
                 reads=[v_t, PTt], writes=[pot])
        P.op("act", lambda: nc.scalar.copy(out=out_ap, in_=po[:, :128]), reads=[pot], writes=[out_t])

    def nq_loop(self):
        return range(min(NQT, self.lim) if self.lim else NQT)

    def phase_mla(self, P, ps, kanT, kpeT, va, qanT, qarT, cmask, oT, oT_base):
        nc = self.nc
        scale = 192.0 ** -0.5
        with ExitStack() as es:
            kn = Ring(P, es, "ml_kn", [128, TK], BF16, 2)
            vh = Ring(P, es, "ml_v", [128, NKT, 128], BF16, 2)
            qn = Ring(P, es, "ml_qn", [128, TQ], BF16, 2)
            qr = Ring(P, es, "ml_qr", [64, TQ], BF16, 2)
            oR = Ring(P, es, "ml_o", [128, TQ], BF16, 2)
            R = dict(st=Ring(P, es, "ml_st", [128, 4], F32, 4), pb=Ring(P, es, "ml_pb", [128, TK], BF16, 2),
                     dg=Ring(P, es, "ml_dg", [128, 128], BF16, 2), pt=Ring(P, es, "ml_pt", [128, NKT, 128], BF16, 2))
            Sr = Ring(P, es, "ml_s", [128, TK], F32, 2)
            kpe = es.enter_context(nc.sbuf_tensor(self.un("ml_kpe"), [64, TK], BF16))
            cm = es.enter_context(nc.sbuf_tensor(self.un("ml_cm"), [128, NQT, 256], F32))
            ct = P.tr("ml_c")
            ct2 = P.tr("ml_c2")
            P.op("sp", lambda: nc.sync.dma_start(out=kpe[:], in_=kpeT.ap()[0]), writes=[ct], dma=ct)
            P.op("sp", lambda: nc.sync.dma_start(out=cm[:], in_=cmask.ap().rearrange("q p c -> p q c")), writes=[ct2], dma=ct2)
            for h in range(16):
                k_, kt_ = kn.next()
                v_, vt_ = vh.next()
                qn_, qnt = qn.next()
                qr_, qrt = qr.next()
                P.op("sp", lambda k_=k_, h=h: nc.sync.dma_start(out=k_[:], in_=kanT.ap()[h]), writes=[kt_], dma=kt_)
                P.op("sp", lambda v_=v_, h=h: nc.sync.dma_start(out=v_[:], in_=va.ap()[:, h * 128:(h + 1) * 128].rearrange("(k p) d -> p k d", p=128)),
                     writes=[vt_], dma=vt_)
                P.op("sp", lambda qn_=qn_, h=h: nc.sync.dma_start(out=qn_[:], in_=qanT.ap()[h]), writes=[qnt], dma=qnt)
                P.op("sp", lambda qr_=qr_, h=h: nc.sync.dma_start(out=qr_[:], in_=qarT.ap()[h]), writes=[qrt], dma=qrt)
                o, ot = oR.next()
                for p in self.nq_loop():
                    nk = min(2 * p + 2, NKT)
                    ncol = nk * 128
                    S, St = Sr.next()
                    for (c0, n) in groups_of(ncol):
                        pt, ptt = ps.next()
                        P.op("pe", lambda pt=pt, p=p, c0=c0, n=n: nc.tensor.matmul(pt[:, :n], lhsT=qn_[:, p * 128:(p + 1) * 128], rhs=k_[:, c0:c0 + n],
                                                                                  start=True, stop=False), reads=[qnt, kt_], writes=[ptt])
                        P.op("pe", lambda pt=pt, p=p, c0=c0, n=n: nc.tensor.matmul(pt[:, :n], lhsT=qr_[:, p * 128:(p + 1) * 128], rhs=kpe[:, c0:c0 + n],
                                                                                  start=False, stop=True), reads=[qrt, ct], writes=[ptt])
                        P.op("act", lambda pt=pt, S=S, c0=c0, n=n: nc.scalar.mul(out=S[:, c0:c0 + n], in_=pt[:, :n], mul=scale), reads=[ptt], writes=[St])
                    P.op("dve", lambda S=S, p=p, ncol=ncol: nc.vector.tensor_tensor(out=S[:, ncol - 256:ncol], in0=S[:, ncol - 256:ncol], in1=cm[:, p, :], op=ALU.add),
                         reads=[St, ct2], writes=[St])
                    self.softmax_pv(P, ps, R, S, St, nk, lambda kt, v_=v_, vt_=vt_: (v_[:, kt, :], vt_), o[:, p * 128:(p + 1) * 128], ot)
                nqc = len(self.nq_loop()) * 128
                P.op("sp", lambda o=o, h=h: nc.sync.dma_start(out=oT.ap()[oT_base + h, :, :nqc], in_=o[:, :nqc]), reads=[ot], dma=ot)
        P.barrier()

    def phase_dsa(self, P, ps, kbT, vb, kixT, qbT, qixT, wix, cmask, oT, oT_base, topk=256):
        nc = self.nc
        scale = 128.0 ** -0.5
        with ExitStack() as es:
            kb = es.enter_context(nc.sbuf_tensor(self.un("ds_kb"), [128, 4, TK], BF16))
            vv = es.enter_context(nc.sbuf_tensor(self.un("ds_v"), [128, NKT, 512], BF16))
            kx = es.enter_context(nc.sbuf_tensor(self.un("ds_kx"), [128, TK], BF16))
            cm = es.enter_context(nc.sbuf_tensor(self.un("ds_cm"), [128, NQT, 256], F32))
            ct = [P.tr("ds_c%d" % i) for i in range(5)]
            P.op("sp", lambda: nc.sync.dma_start(out=kb[:], in_=kbT.ap().rearrange("g p t -> p g t")), writes=[ct[0]], dma=ct[0])
            P.op("sp", lambda: nc.sync.dma_start(out=vv[:], in_=vb.ap().rearrange("(k p) d -> p k d", p=128)), writes=[ct[1]], dma=ct[1])
            P.op("sp", lambda: nc.sync.dma_start(out=kx[0:64, :], in_=kixT.ap()[0]), writes=[ct[2]], dma=ct[2])
            P.op("sp", lambda: nc.sync.dma_start(out=kx[64:128, :], in_=kixT.ap()[0]), writes=[ct[3]], dma=ct[3])
            P.op("sp", lambda: nc.sync.dma_start(out=cm[:], in_=cmask.ap().rearrange("q p c -> p q c")), writes=[ct[4]], dma=ct[4])
            qxr = Ring(P, es, "ds_qx", [128, 8, 128], BF16, 2)
            qbr = Ring(P, es, "ds_qb", [128, 16, 128], BF16, 2)
            wr = Ring(P, es, "ds_w", [128, 16], F32, 2)
            rr = Ring(P, es, "ds_r", [128, 512], F32, 3)
            acc = es.enter_context(nc.sbuf_tensor(self.un("ds_acc"), [128, TK], F32))
            acct = P.tr("ds_acc")
            W0 = es.enter_context(nc.sbuf_tensor(self.un("ds_w0"), [128, TK], F32))
            W0t = P.tr("ds_w0")
            W1 = es.enter_context(nc.sbuf_tensor(self.un("ds_w1"), [128, TK], F32))
            W1t = P.tr("ds_w1")
            m8r = Ring(P, es, "ds_m8", [128, 8], F32, 4)
            thr = Ring(P, es, "ds_thr", [128, 2], F32, 2)
            oR = Ring(P, es, "ds_o", [128, 16, 128], BF16, 2)
            R = dict(st=Ring(P, es, "ds_st", [128, 4], F32, 4), pb=Ring(P, es, "ds_pb", [128, TK], BF16, 1),
                     dg=Ring(P, es, "ds_dg", [128, 128], BF16, 2), pt=Ring(P, es, "ds_pt", [128, NKT, 128], BF16, 1))
            for p in self.nq_loop():
                nk = min(2 * p + 2, NKT)
                ncol = nk * 128
                qx, qxt = qxr.next()
                qb, qbt = qbr.next()
                w_, wt_ = wr.next()
                P.op("sp", lambda qx=qx, p=p: nc.sync.dma_start(out=qx[:], in_=qixT.ap()[:, :, p * 128:(p + 1) * 128].rearrange("k p t -> p k t")),
                     writes=[qxt], dma=qxt)
                P.op("sp", lambda qb=qb, p=p: nc.sync.dma_start(out=qb[:], in_=qbT.ap()[:, :, p * 128:(p + 1) * 128].rearrange("k p t -> p k t")),
                     writes=[qbt], dma=qbt)
                P.op("sp", lambda w_=w_, p=p: nc.sync.dma_start(out=w_[:], in_=wix.ap()[p * 128:(p + 1) * 128, :]), writes=[wt_], dma=wt_)
                for hi in range(16):
                    b, r0 = hi // 2, (hi % 2) * 64
                    for (c0, n) in groups_of(ncol):
                        pt, ptt = ps.next()
                        P.op("pe", lambda pt=pt, qx=qx, b=b, r0=r0, c0=c0, n=n: nc.tensor.matmul(
                            pt[:, :n], lhsT=qx[r0:r0 + 64, b, :], rhs=kx[r0:r0 + 64, c0:c0 + n], start=True, stop=True),
                            reads=[qxt, ct[2], ct[3]], writes=[ptt])
                        r, rt = rr.next()
                        P.op("act", lambda r=r, pt=pt, n=n: nc.scalar.activation(out=r[:, :n], in_=pt[:, :n], func=AF.Relu), reads=[ptt], writes=[rt])
                        if hi == 0:
                            P.op("dve", lambda r=r, w_=w_, c0=c0, n=n, hi=hi: nc.vector.tensor_scalar(
                                out=acc[:, c0:c0 + n], in0=r[:, :n], scalar1=w_[:, hi:hi + 1], scalar2=None, op0=ALU.mult),
                                reads=[rt, wt_], writes=[acct])
                        else:
                            P.op("dve", lambda r=r, w_=w_, c0=c0, n=n, hi=hi: nc.vector.scalar_tensor_tensor(
                                out=acc[:, c0:c0 + n], in0=r[:, :n], scalar=w_[:, hi:hi + 1], op0=ALU.mult, in1=acc[:, c0:c0 + n], op1=ALU.add),
                                reads=[rt, wt_, acct], writes=[acct])
                P.op("dve", lambda p=p, ncol=ncol: nc.vector.tensor_tensor(out=acc[:, ncol - 256:ncol], in0=acc[:, ncol - 256:ncol], in1=cm[:, p, :], op=ALU.add),
                     reads=[acct, ct[4]], writes=[acct])
                th, tht = thr.next()
                if ncol > topk:
                    cur, curt = acc, acct
                    bufs = [(W0, W0t), (W1, W1t)]
                    nr = topk // 8
                    for r_i in range(nr):
                        m8, m8t = m8r.next()
                        P.op("dve", lambda m8=m8, cur=cur, ncol=ncol: nc.vector.max(out=m8[:], in_=cur[:, :ncol]), reads=[curt], writes=[m8t])
                        if r_i == nr - 1:
                            P.op("dve", lambda m8=m8, th=th: nc.vector.tensor_scalar(out=th[:, 0:1], in0=m8[:, 7:8], scalar1=-1.0e29, scalar2=None, op0=ALU.max),
                                 reads=[m8t], writes=[tht])
                            break
                        nxt, nxtt = bufs[r_i % 2]
                        P.op("dve", lambda m8=m8, cur=cur, nxt=nxt, ncol=ncol: nc.vector.match_replace(
                            out=nxt[:, :ncol], in_to_replace=m8[:], in_values=cur[:, :ncol], imm_value=NEG), reads=[curt, m8t], writes=[nxtt])
                        cur, curt = nxt, nxtt
                else:
                    P.op("dve", lambda th=th: nc.vector.memset(th[:, 0:1], -1.0e29), writes=[tht])
                P.op("dve", lambda th=th, ncol=ncol: nc.vector.tensor_scalar(out=W1[:, :ncol], in0=acc[:, :ncol], scalar1=th[:, 0:1], scalar2=NEG,
                                                                             op0=ALU.is_lt, op1=ALU.mult), reads=[acct, tht], writes=[W1t])
                o, ot = oR.next()
                for h in range(16):
                    g = h // 4
                    S, St = W0, W0t
                    for (c0, n) in groups_of(ncol):
                        pt, ptt = ps.next()
                        P.op("pe", lambda pt=pt, qb=qb, h=h, g=g, c0=c0, n=n: nc.tensor.matmul(pt[:, :n], lhsT=qb[:, h, :], rhs=kb[:, g, c0:c0 + n],
                                                                                          start=True, stop=True), reads=[qbt, ct[0]], writes=[ptt])
                        P.op("dve", lambda pt=pt, c0=c0, n=n: nc.vector.scalar_tensor_tensor(out=W0[:, c0:c0 + n], in0=pt[:, :n], scalar=scale, op0=ALU.mult,
                                                                                              in1=W1[:, c0:c0 + n], op1=ALU.add), reads=[ptt, W1t], writes=[W0t])
                    self.softmax_pv(P, ps, R, S, St, nk, lambda kt, g=g: (vv[:, kt, g * 128:(g + 1) * 128], ct[1]), o[:, h, :], ot)
                P.op("sp", lambda o=o, p=p: nc.sync.dma_start(out=oT.ap()[oT_base:oT_base + 16, :, p * 128:(p + 1) * 128].rearrange("k p t -> p k t"), in_=o[:]),
                     reads=[ot], dma=ot)
        P.barrier()

    def phase_peer_gates(self, P, ps, pqT, subk, gT):
        nc = self.nc
        with ExitStack() as es:
            keys = es.enter_context(nc.sbuf_tensor(self.un("pg_k"), [128, 16, 128], BF16))
            kt_ = P.tr("pg_k")
            P.op("pool", lambda: nc.gpsimd.dma_start(out=keys[:], in_=subk.ap()), writes=[kt_], dma=kt_)
            pqr = Ring(P, es, "pg_q", [128, 16, 128], BF16, 2)
            sr = Ring(P, es, "pg_s", [128, 16, 128], F32, 2)
            wr = Ring(P, es, "pg_w", [128, 16, 128], F32, 1)
            m8r = Ring(P, es, "pg_m8", [128, 16, 16], F32, 2)
            cr = Ring(P, es, "pg_c", [128, 8, 256], F32, 1)
            cwr = Ring(P, es, "pg_cw", [128, 8, 256], F32, 1)
            c8r = Ring(P, es, "pg_c8", [128, 8, 24], F32, 2)
            smr = Ring(P, es, "pg_sm", [128, 8, 8], F32, 2)
            e16r = Ring(P, es, "pg_e16", [128, 8, 16], F32, 2)
            nAr = Ring(P, es, "pg_nA", [128, 8, 128], F32, 2)
            E1r = Ring(P, es, "pg_E1", [128, 8, 128], BF16, 2)
            E2r = Ring(P, es, "pg_E2", [128, 8, 128], BF16, 2)
            tmr = Ring(P, es, "pg_tm", [128, 8, 128], F32, 2)
            mkr = Ring(P, es, "pg_mk", [128, 8, 128], BF16, 3)
            Mr = Ring(P, es, "pg_M", [128, 8, 128], BF16, 3)
            gsr = Ring(P, es, "pg_gs", [128, 4, 128], BF16, 3)
            for tl in self.nq_loop():
                pq, pqt = pqr.next()
                P.op("sp", lambda pq=pq, tl=tl: nc.sync.dma_start(out=pq[:], in_=pqT.ap()[:, :, tl * 128:(tl + 1) * 128].rearrange("k p t -> p k t")),
                     writes=[pqt], dma=pqt)
                s, st = sr.next()
                for q4 in range(4):
                    pt, ptt = ps.next()
                    for j in range(4):
                        hc = q4 * 4 + j
                        P.op("pe", lambda pt=pt, pq=pq, hc=hc, j=j: nc.tensor.matmul(pt[:, j * 128:(j + 1) * 128], lhsT=pq[:, hc, :], rhs=keys[:, hc, :],
                                                                                    start=True, stop=True), reads=[pqt, kt_], writes=[ptt])
                    P.op("act", lambda pt=pt, s=s, q4=q4: nc.scalar.copy(out=s[:, q4 * 4:(q4 + 1) * 4, :], in_=pt[:, :].rearrange("p (c n) -> p c n", c=4)),
                         reads=[ptt], writes=[st])
                w, wt = wr.next()
                m8, m8t = m8r.next()
                for hc in range(16):
                    P.op("dve", lambda m8=m8, s=s, hc=hc: nc.vector.max(out=m8[:, hc, 0:8], in_=s[:, hc, :]), reads=[st], writes=[m8t])
                    P.op("dve", lambda m8=m8, s=s, w=w, hc=hc: nc.vector.match_replace(out=w[:, hc, :], in_to_replace=m8[:, hc, 0:8], in_values=s[:, hc, :],
                                                                                      imm_value=NEG), reads=[st, m8t], writes=[wt])
                    P.op("dve", lambda m8=m8, w=w, hc=hc: nc.vector.max(out=m8[:, hc, 8:16], in_=w[:, hc, :]), reads=[wt], writes=[m8t])
                c, ct = cr.next()
                for h in range(8):
                    P.op("dve", lambda c=c, m8=m8, h=h: nc.vector.tensor_tensor(
                        out=c[:, h, :].rearrange("p (a b) -> p a b", a=16),
                        in0=m8[:, 2 * h, :].unsqueeze(2).to_broadcast([128, 16, 16]),
                        in1=m8[:, 2 * h + 1, :].unsqueeze(1).to_broadcast([128, 16, 16]), op=ALU.add), reads=[m8t], writes=[ct])
                cw, cwt = cwr.next()
                c8, c8t = c8r.next()
                for h in range(8):
                    P.op("dve", lambda c8=c8, c=c, h=h: nc.vector.max(out=c8[:, h, 0:8], in_=c[:, h, :]), reads=[ct], writes=[c8t])
                    P.op("dve", lambda c8=c8, c=c, cw=cw, h=h: nc.vector.match_replace(out=cw[:, h, :], in_to_replace=c8[:, h, 0:8], in_values=c[:, h, :],
                                                                                      imm_value=NEG), reads=[ct, c8t], writes=[cwt])
                    P.op("dve", lambda c8=c8, cw=cw, h=h: nc.vector.max(out=c8[:, h, 8:16], in_=cw[:, h, :]), reads=[cwt], writes=[c8t])
                    P.op("dve", lambda c8=c8, cw=cw, c=c, h=h: nc.vector.match_replace(out=c[:, h, :], in_to_replace=c8[:, h, 8:16], in_values=cw[:, h, :],
                                                                                      imm_value=NEG), reads=[cwt, c8t], writes=[ct])
                    P.op("dve", lambda c8=c8, c=c, h=h: nc.vector.max(out=c8[:, h, 16:24], in_=c[:, h, :]), reads=[ct], writes=[c8t])
                sm, smt = smr.next()
                P.op("dve", lambda sm=sm, c8=c8: nc.vector.tensor_tensor(out=sm[:, :, 0:1], in0=c8[:, :, 15:16], in1=c8[:, :, 16:17], op=ALU.add),
                     reads=[c8t], writes=[smt])
                P.op("dve", lambda sm=sm: nc.vector.tensor_scalar(out=sm[:, :, 0:1], in0=sm[:, :, 0:1], scalar1=0.5, scalar2=None, op0=ALU.mult),
                     reads=[smt], writes=[smt])
                e16, e16t = e16r.next()
                P.op("dve", lambda e16=e16, c8=c8: nc.vector.tensor_tensor(out=e16[:], in0=c8[:, :, 0:16], in1=c8[:, :, 0:1].to_broadcast([128, 8, 16]),
                                                                           op=ALU.subtract), reads=[c8t], writes=[e16t])
                P.op("act", lambda e16=e16: nc.scalar.activation(out=e16[:], in_=e16[:], func=AF.Exp), reads=[e16t], writes=[e16t])
                P.op("dve", lambda sm=sm, e16=e16: nc.vector.tensor_reduce(out=sm[:, :, 1:2], in_=e16[:], op=ALU.add, axis=AX.X), reads=[e16t, smt], writes=[smt])
                P.op("dve", lambda sm=sm: nc.vector.reciprocal(out=sm[:, :, 2:3], in_=sm[:, :, 1:2]), reads=[smt], writes=[smt])
                sv = s[:, :, :].rearrange("p (h c) n -> p h c n", c=2)
                m8v = m8[:, :, :].rearrange("p (h c) k -> p h c k", c=2)
                nA, nAt = nAr.next()
                P.op("dve", lambda nA=nA, sm=sm, sv=sv: nc.vector.tensor_tensor(out=nA[:], in0=sm[:, :, 0:1].to_broadcast([128, 8, 128]), in1=sv[:, :, 0, :],
                                                                                op=ALU.subtract), reads=[smt, st], writes=[nAt])
                E1, E1t = E1r.next()
                E2, E2t = E2r.next()
                t1, t1t = tmr.next()
                P.op("dve", lambda t1=t1, sv=sv, m8v=m8v: nc.vector.tensor_tensor(out=t1[:], in0=sv[:, :, 0, :], in1=m8v[:, :, 0, 0:1].to_broadcast([128, 8, 128]),
                                                                                  op=ALU.subtract), reads=[st, m8t], writes=[t1t])
                P.op("act", lambda t1=t1: nc.scalar.activation(out=t1[:], in_=t1[:], func=AF.Exp), reads=[t1t], writes=[t1t])
                P.op("dve", lambda E1=E1, t1=t1, sm=sm: nc.vector.tensor_tensor(out=E1[:], in0=t1[:], in1=sm[:, :, 2:3].to_broadcast([128, 8, 128]), op=ALU.mult),
                     reads=[t1t, smt], writes=[E1t])
                t2, t2t = tmr.next()
                P.op("dve", lambda t2=t2, sv=sv, m8v=m8v: nc.vector.tensor_tensor(out=t2[:], in0=sv[:, :, 1, :], in1=m8v[:, :, 1, 0:1].to_broadcast([128, 8, 128]),
                                                                                  op=ALU.subtract), reads=[st, m8t], writes=[t2t])
                P.op("act", lambda t2=t2, E2=E2: nc.scalar.activation(out=E2[:], in_=t2[:], func=AF.Exp), reads=[t2t], writes=[E2t])
                for i4 in range(32):
                    pt, ptt = ps.next()
                    for j in range(4):
                        i = i4 * 4 + j
                        mk, mkt = mkr.next()
                        P.op("dve", lambda mk=mk, sv=sv, nA=nA, i=i: nc.vector.tensor_tensor(out=mk[:], in0=sv[:, :, 1, :], in1=nA[:, :, i:i + 1].to_broadcast([128, 8, 128]),
                                                                                             op=ALU.is_ge), reads=[st, nAt], writes=[mkt])
                        P.op("dve", lambda mk=mk, E2=E2: nc.vector.tensor_tensor(out=mk[:], in0=mk[:], in1=E2[:], op=ALU.mult), reads=[mkt, E2t], writes=[mkt])
                        M, Mt = Mr.next()
                        P.op("dve", lambda M=M, mk=mk, E1=E1, i=i: nc.vector.tensor_tensor(out=M[:], in0=mk[:], in1=E1[:, :, i:i + 1].to_broadcast([128, 8, 128]),
                                                                                           op=ALU.mult), reads=[mkt, E1t], writes=[Mt])
                        for h in range(8):
                            P.op("pe", lambda pt=pt, M=M, h=h, j=j: nc.tensor.matmul(pt[:, j * 128:(j + 1) * 128], lhsT=M[:, h, :], rhs=self.ident[:],
                                                                                    start=(h == 0), stop=(h == 7)), reads=[Mt, self.const_t], writes=[ptt])
                    gs, gst = gsr.next()
                    P.op("act", lambda gs=gs, pt=pt: nc.scalar.copy(out=gs[:], in_=pt[:, :].rearrange("p (c t) -> p c t", c=4)), reads=[ptt], writes=[gst])
                    P.op("sp", lambda gs=gs, i4=i4, tl=tl: nc.sync.dma_start(
                        out=gT.ap()[i4 * 4:(i4 + 1) * 4, :, tl * 128:(tl + 1) * 128].rearrange("i j t -> j i t"), in_=gs[:]), reads=[gst], dma=gst)
        P.barrier()

    def phase_peer_main(self, P, ps, hn2T, Ud, Vd, gT, po_d):
        nc = self.nc
        nchunk = 128
        if self.lim:
            nchunk = 4
        with ExitStack() as es:
            xr = Ring(P, es, "pm_x", [128, 32, 512], BF16, 1)
            accr = Ring(P, es, "pm_acc", [128, 4, D], F32, 1)
            ur = Ring(P, es, "pm_u", [128, D], BF16, 2)
            utr = Ring(P, es, "pm_ut", [128, 32, 128], BF16, 2)
            vr = Ring(P, es, "pm_v", [128, D], BF16, 4)
            gr = Ring(P, es, "pm_g", [128, 512], BF16, 3)
            wr = Ring(P, es, "pm_w", [128, 512], BF16, 4)
            t1r = Ring(P, es, "pm_t1", [128, 512], F32, 2)
            t2r = Ring(P, es, "pm_t2", [128, 512], F32, 2)
            tgs = groups_of(TQ)
            if self.lim:
                tgs = tgs[:1]
            for (t0, n) in tgs:
                ntl = n // 128
                x, xt = xr.next()
                for k0 in (0, 16):
                    P.op("sp", lambda x=x, t0=t0, n=n, k0=k0: nc.sync.dma_start(
                        out=x[:, k0:k0 + 16, :n], in_=hn2T.ap()[k0:k0 + 16, :, t0:t0 + n].rearrange("k p t -> p k t")), writes=[xt], dma=xt)
                acc, acct = accr.next()
                for ip in range(nchunk // 2):
                    Ws = []
                    Vs = []
                    for c in range(2):
                        i = ip * 2 + c
                        u, ut = ur.next()
                        P.op("pool", lambda u=u, i=i: nc.gpsimd.dma_start(out=u[:], in_=Ud.ap()[i * 128:(i + 1) * 128, :], max_dma_last_dim=8192),
                             writes=[ut], dma=ut)
                        v, vt = vr.next()
                        P.op("pool", lambda v=v, i=i: nc.gpsimd.dma_start(out=v[:], in_=Vd.ap()[i * 128:(i + 1) * 128, :], max_dma_last_dim=8192),
                             writes=[vt], dma=vt)
                        g, gt = gr.next()
                        P.op("sp", lambda g=g, i=i, t0=t0, n=n: nc.sync.dma_start(out=g[:, :n], in_=gT.ap()[i, :, t0:t0 + n]), writes=[gt], dma=gt)
                        UT, UTt = utr.next()
                        for kq in range(8):
                            pt, ptt = ps.next()
                            for j in range(4):
                                kc = kq * 4 + j
                                P.op("pe", lambda pt=pt, u=u, kc=kc, j=j: nc.tensor.matmul(pt[:, j * 128:(j + 1) * 128], lhsT=u[:, kc * 128:(kc + 1) * 128],
                                                                                          rhs=self.ident[:], start=True, stop=True), reads=[ut, self.const_t], writes=[ptt])
                            dst = UT[:, kq * 4:(kq + 1) * 4, :]
                            src = pt[:, :].rearrange("p (c t) -> p c t", c=4)
                            if kq % 2 == 0:
                                P.op("act", lambda dst=dst, src=src: nc.scalar.copy(out=dst, in_=src), reads=[ptt], writes=[UTt])
                            else:
                                P.op("dve", lambda dst=dst, src=src: nc.vector.tensor_copy(out=dst, in_=src), reads=[ptt], writes=[UTt])
                        pa, pat = ps.next()
                        for kc in range(32):
                            P.op("pe", lambda pa=pa, UT=UT, x=x, kc=kc, n=n: nc.tensor.matmul(pa[:, :n], lhsT=UT[:, kc, :], rhs=x[:, kc, :n],
                                                                                             start=(kc == 0), stop=(kc == 31)), reads=[UTt, xt], writes=[pat])
                        t1, t1t = t1r.next()
                        t2, t2t = t2r.next()
                        P.op("act", lambda t1=t1, pa=pa, n=n: nc.scalar.activation(out=t1[:, :n], in_=pa[:, :n], func=AF.Square), reads=[pat], writes=[t1t])
                        P.op("dve", lambda t1=t1, n=n: nc.vector.tensor_scalar(out=t1[:, :n], in0=t1[:, :n], scalar1=0.044715, scalar2=1.0, op0=ALU.mult, op1=ALU.add),
                             reads=[t1t], writes=[t1t])
                        P.op("dve", lambda t1=t1, t2=t2, pa=pa, n=n: nc.vector.tensor_tensor(out=t2[:, :n], in0=pa[:, :n], in1=t1[:, :n], op=ALU.mult),
                             reads=[pat, t1t], writes=[t2t])
                        P.op("act", lambda t2=t2, n=n: nc.scalar.activation(out=t2[:, :n], in_=t2[:, :n], func=AF.Sigmoid, scale=1.5957691216057308),
                             reads=[t2t], writes=[t2t])
                        P.op("dve", lambda t1=t1, t2=t2, pa=pa, n=n: nc.vector.tensor_tensor(out=t1[:, :n], in0=pa[:, :n], in1=t2[:, :n], op=ALU.mult),
                             reads=[pat, t2t], writes=[t1t])
                        W, Wt = wr.next()
                        P.op("dve", lambda W=W, t1=t1, g=g, n=n: nc.vector.tensor_tensor(out=W[:, :n], in0=t1[:, :n], in1=g[:, :n], op=ALU.mult),
                             reads=[t1t, gt], writes=[Wt])
                        Ws.append((W, Wt))
                        Vs.append((v, vt))
                    for tt in range(ntl):
                        for db in range(8):
                            po, pot = ps.next()
                            for c in range(2):
                                W, Wt = Ws[c]
                                v, vt = Vs[c]
                                P.op("pe", lambda po=po, W=W, v=v, tt=tt, db=db, c=c: nc.tensor.matmul(
                                    po[:, :], lhsT=W[:, tt * 128:(tt + 1) * 128], rhs=v[:, db * 512:(db + 1) * 512], start=(c == 0), stop=(c == 1)),
                                    reads=[Wt, vt], writes=[pot])
                            dst = acc[:, tt, db * 512:(db + 1) * 512]
                            if ip == 0:
                                P.op("act", lambda dst=dst, po=po: nc.scalar.copy(out=dst, in_=po[:, :]), reads=[pot], writes=[acct])
                            else:
                                P.op("dve", lambda dst=dst, po=po: nc.vector.tensor_tensor(out=dst, in0=po[:, :], in1=dst, op=ALU.add), reads=[pot, acct], writes=[acct])
                for tt in range(ntl):
                    P.op("sp", lambda acc=acc, tt=tt, t0=t0: nc.sync.dma_start(out=po_d.ap()[t0 + tt * 128:t0 + (tt + 1) * 128, :], in_=acc[:, tt, :]),
                         reads=[acct], dma=acct)
        P.barrier()

    def phase_final(self, P, ps, h1, po_d, g_ap, y):
        nc = self.nc
        with ExitStack() as es:
            xin = Ring(P, es, "fi_x", [128, D], F32, 2)
            xad = Ring(P, es, "fi_a", [128, D], F32, 2)
            yr = Ring(P, es, "fi_y", [128, D], F32, 2)
            junk = es.enter_context(nc.sbuf_tensor(self.un("fi_junk"), [128, D], BF16))
            junk_t = P.tr("fi_junk")
            gbc = es.enter_context(nc.sbuf_tensor(self.un("fi_g"), [128, D], F32))
            gbc_t = P.tr("fi_gbc")
            st = Ring(P, es, "fi_st", [128, 4], F32, 4)
            P.op("sp", lambda: nc.sync.dma_start(out=gbc[:], in_=g_ap.partition_broadcast(128)), writes=[gbc_t], dma=gbc_t)
            for i in self.nq_loop():
                x, xt = xin.next()
                a, at = xad.next()
                P.op("sp", lambda x=x, i=i: nc.sync.dma_start(out=x[:], in_=h1.ap()[i * 128:(i + 1) * 128, :]), writes=[xt], dma=xt)
                P.op("sp", lambda a=a, i=i: nc.sync.dma_start(out=a[:], in_=po_d.ap()[i * 128:(i + 1) * 128, :]), writes=[at], dma=at)
                P.op("dve", lambda x=x, a=a: nc.vector.tensor_tensor(out=x[:], in0=x[:], in1=a[:], op=ALU.add), reads=[at, xt], writes=[xt])
                s, s_t = st.next()
                P.op("act", lambda x=x, s=s: nc.scalar.activation(out=junk[:], in_=x[:], func=AF.Square, accum_out=s[:, 0:1]), reads=[xt], writes=[junk_t, s_t])
                P.op("act", lambda s=s: nc.scalar.activation(out=s[:, 1:2], in_=s[:, 0:1], func=AF.Sqrt, scale=1.0 / D, bias=self.eps_ap),
                     reads=[s_t, self.const_t], writes=[s_t])
                P.op("dve", lambda s=s: nc.vector.reciprocal(out=s[:, 2:3], in_=s[:, 1:2]), reads=[s_t], writes=[s_t])
                yt_, ytt = yr.next()
                P.op("dve", lambda x=x, s=s, yt_=yt_: nc.vector.scalar_tensor_tensor(out=yt_[:], in0=x[:], scalar=s[:, 2:3], op0=ALU.mult, in1=gbc[:], op1=ALU.mult),
                     reads=[xt, s_t, gbc_t], writes=[ytt])
                P.op("sp", lambda yt_=yt_, i=i: nc.sync.dma_start(out=y.ap()[i * 128:(i + 1) * 128, :], in_=yt_[:]), reads=[ytt], dma=ytt)
        P.barrier()

    def begin(self, es, consts):
        nc = self.nc
        P = Prog(nc, es)
        self.P = P
        ps = Ring(P, es, "ps", [128, 512], F32, 8, psum=True)
        self.const_t = P.tr("const")
        self.ident = es.enter_context(nc.sbuf_tensor(self.un("ident"), [128, 128], BF16))
        self.ones = es.enter_context(nc.sbuf_tensor(self.un("ones"), [128, 128], BF16))
        self.cst = es.enter_context(nc.sbuf_tensor(self.un("cst"), [128, 64], F32))
        identf = es.enter_context(nc.sbuf_tensor(self.un("identf"), [128, 128], F32))
        self.eps_ap = self.cst[:, 63:64]
        P.op("pool", lambda: nc.gpsimd.memset(identf[:], 1.0), writes=[self.const_t])
        P.op("pool", lambda: nc.gpsimd.affine_select(out=identf[:], in_=identf[:], pattern=[[-1, 128]], compare_op=ALU.is_equal,
                                                    fill=0.0, base=0, channel_multiplier=1), reads=[self.const_t], writes=[self.const_t])
        P.op("pool", lambda: nc.gpsimd.tensor_copy(out=self.ident[:], in_=identf[:]), reads=[self.const_t], writes=[self.const_t])
        P.op("pool", lambda: nc.gpsimd.memset(self.ones[:], 1.0), writes=[self.const_t])
        P.op("sp", lambda: nc.sync.dma_start(out=self.cst[:], in_=consts.ap()), writes=[self.const_t], dma=self.const_t)
        P.barrier()
        return P, ps

    def build(self):
        nc = self.nc
        din, dscr = self.din, self.dscr
        NP = self.nphase
        hall = din("hall", [TK, D])
        hq = din("hq", [TQ, D])
        g_attn = din("g_attn", [D])
        consts = din("consts", [128, 64])
        hnTk = dscr("hnTk", [32, 128, TK])
        hnTq = dscr("hnTq", [32, 128, TQ])
        yout = nc.dram_tensor("y", [TQ, D], F32, kind="ExternalOutput")
        phases = []
        phases.append(lambda P, ps: self.phase_rownorm_T(P, ps, [(hall, hnTk, TK), (hq, hnTq, TQ)], g_attn.ap()))
        if NP > 1:
            wk = din("wk", [D, 1872])
            ropek = din("ropek", [6, 128, TK])
            ckvT = dscr("ckvT", [4, 128, TK]); ckvnT = dscr("ckvnT", [4, 128, TK]); kpeT = dscr("kpeT", [1, 64, TK])
            kbT = dscr("kbT", [4, 128, TK]); kixT = dscr("kixT", [1, 64, TK]); vb = dscr("vb", [TK, 512])
            ku = [dict(kind="plain", c0=i * 128, m=128, dst=ckvT, idx=i) for i in range(4)]
            ku.append(dict(kind="rope", c0=512, m=64, mb=64, dst=kpeT, idx=0, ti=0, rows=[(0, 64)]))
            for g in range(4):
                ku.append(dict(kind="rope", c0=640 + g * 160, m=128, mb=32, dst=kbT, idx=g, ti=1, rows=[(0, 32)]))
            ku.append(dict(kind="rope", c0=1280, m=64, mb=16, dst=kixT, idx=0, ti=2, rows=[(0, 16)]))
            ku.append(dict(kind="tok", c0=1360, m=512, dst=vb, col=0, dtype=BF16))
            phases.append(lambda P, ps: self.phase_gemm(P, ps, wk, D, hnTk, TK, ku, rope=ropek))
            phases.append(lambda P, ps: self.phase_fnorm(P, ps, [(ckvT, ckvnT, 4, TK, self.cst[:, 0:4])]))
        if NP > 3:
            wukv2 = din("wukv2", [512, 4096])
            kanT = dscr("kanT", [16, 128, TK]); va = dscr("va", [TK, 2048])
            u2 = [dict(kind="plain", c0=h * 128, m=128, dst=kanT, idx=h) for h in range(16)]
            u2 += [dict(kind="tok", c0=2048 + j * 512, m=512, dst=va, col=j * 512, dtype=BF16) for j in range(4)]
            phases.append(lambda P, ps: self.phase_gemm(P, ps, wukv2, 512, ckvnT, TK, u2))
        if NP > 4:
            wq = din("wq", [D, 13456])
            ropeq = din("ropeq", [6, 128, TQ])
            cqT = dscr("cqT", [8, 128, TQ]); cqnT = dscr("cqnT", [8, 128, TQ]); qbT = dscr("qbT", [16, 128, TQ]); qixT = dscr("qixT", [8, 128, TQ])
            wix = dscr("wix", [TQ, 16], F32); gaT = dscr("gaT", [32, 128, TQ]); gbT = dscr("gbT", [32, 128, TQ])
            qu = [dict(kind="plain", c0=i * 128, m=128, dst=cqT, idx=i) for i in range(8)]
            qu += [dict(kind="rope", c0=1024 + h * 160, m=128, mb=32, dst=qbT, idx=h, ti=1, rows=[(0, 32)]) for h in range(16)]
            qu += [dict(kind="rope", c0=3584 + b * 208, m=128, mb=80, dst=qixT, idx=b, ti=2, rows=[(0, 16), (64, 16)]) for b in range(8)]
            qu.append(dict(kind="tok", c0=5248, m=16, dst=wix, col=0, dtype=F32))
            qu += [dict(kind="sig", c0=5264 + j * 128, m=128, dst=gaT, idx=j) for j in range(32)]
            qu += [dict(kind="sig", c0=9360 + j * 128, m=128, dst=gbT, idx=j) for j in range(32)]
            phases.append(lambda P, ps: self.phase_gemm(P, ps, wq, D, hnTq, TQ, qu, rope=ropeq))
            phases.append(lambda P, ps: self.phase_fnorm(P, ps, [(cqT, cqnT, 8, TQ, self.cst[:, 4:12])]))
        if NP > 6:
            wuq2 = din("wuq2", [1024, 4096])
            qanT = dscr("qanT", [16, 128, TQ]); qarT = dscr("qarT", [16, 64, TQ])
            u4 = []
            for h in range(16):
                u4.append(dict(kind="plain", c0=h * 256, m=128, dst=qanT, idx=h))
                u4.append(dict(kind="rope", c0=h * 256 + 128, m=64, mb=64, dst=qarT, idx=h, ti=0, rows=[(0, 64)]))
            phases.append(lambda P, ps: self.phase_gemm(P, ps, wuq2, 1024, cqnT, TQ, u4, rope=ropeq))
        if NP > 7:
            cmask = din("cmask", [NQT, 128, 256])
            oT = dscr("oT", [32, 128, TQ])
            phases.append(lambda P, ps: self.phase_mla(P, ps, kanT, kpeT, va, qanT, qarT, cmask, oT, 0))
        if NP > 8:
            phases.append(lambda P, ps: self.phase_dsa(P, ps, kbT, vb, kixT, qbT, qixT, wix, cmask, oT, 16))
        if NP > 9:
            wbr = din("wbr", [D, D])
            mT = dscr("mT", [32, 128, TQ])
            u5 = [dict(kind="branch", c0=j * 128, m=128, dst=mT, idx=j) for j in range(32)]
            phases.append(lambda P, ps: self.phase_gemm(P, ps, wbr, D, oT, TQ, u5, gates=(gaT, gbT)))
        if NP > 10:
            wout = din("wout", [D, D])
            h1 = dscr("h1", [TQ, D], F32)
            u6 = [dict(kind="tok", c0=j * 512, m=512, dst=h1, col=j * 512, dtype=F32, res=hq) for j in range(8)]
            phases.append(lambda P, ps: self.phase_gemm(P, ps, wout, D, mT, TQ, u6))
        if NP > 11:
            g_ffn = din("g_ffn", [D])
            hn2T = dscr("hn2T", [32, 128, TQ])
            phases.append(lambda P, ps: self.phase_rownorm_T(P, ps, [(h1, hn2T, TQ)], g_ffn.ap()))
        if NP > 12:
            wpq = din("wpq", [D, 2048])
            pqT = dscr("pqT", [16, 128, TQ])
            u7 = [dict(kind="plain", c0=j * 128, m=128, dst=pqT, idx=j) for j in range(16)]
            phases.append(lambda P, ps: self.phase_gemm(P, ps, wpq, D, hn2T, TQ, u7))
        if NP > 13:
            subk = din("subk", [128, 16, 128])
            gT = dscr("gT", [128, 128, TQ])
            phases.append(lambda P, ps: self.phase_peer_gates(P, ps, pqT, subk, gT))
        if NP > 14:
            Ud = din("peer_u", [16384, D])
            Vd = din("peer_v", [16384, D])
            po_d = dscr("po", [TQ, D], F32)
            phases.append(lambda P, ps: self.phase_peer_main(P, ps, hn2T, Ud, Vd, gT, po_d))
        if NP > 15:
            g_fin = din("g_fin", [D])
            phases.append(lambda P, ps: self.phase_final(P, ps, h1, po_d, g_fin.ap(), yout))

        with ExitStack() as es:
            P, ps = self.begin(es, consts)
            for f in phases[:NP]:
                f(P, ps)
            P.barrier()
            nw = P.emit()
            self.stats = (len(P.ops), nw)
        return nc


def rope_tab(pos, rot):
    inv = (THETA ** (-np.arange(0, rot, 2, dtype=np.float32) / np.float32(rot))).astype(np.float32)
    ang = pos.astype(np.float32)[:, None] * inv[None, :]
    return np.cos(ang).astype(np.float32), np.sin(ang).astype(np.float32)


def rope_tables(pos):
    T = len(pos)
    out = np.zeros((6, 128, T), np.float32)
    c, s = rope_tab(pos, 64)
    out[0, 0:32] = c.T; out[0, 32:64] = c.T; out[1, 0:32] = -s.T; out[1, 32:64] = s.T
    c, s = rope_tab(pos, 32)
    out[2, 0:16] = c.T; out[2, 16:32] = c.T; out[3, 0:16] = -s.T; out[3, 16:32] = s.T
    c, s = rope_tab(pos, 16)
    for b in (0, 64):
        out[4, b:b + 8] = c.T; out[4, b + 8:b + 16] = c.T; out[5, b:b + 8] = -s.T; out[5, b + 8:b + 16] = s.T
    return out


def swap_halves(w, rot):
    h = rot // 2
    return np.concatenate([w[:, h:rot], w[:, :h]], axis=1)


OFF = dict(cq=0, ckv=1024, kpe=1536, qb=1600, kb=3648, vb=4160, qix=4672, kix=5696, wix=5760, ga=5776, gb=9872)


def core_tiles(hf):
    return [min(2 * p + hf, 32) if (2 * p + hf) <= 32 else 31 for p in range(NQT)]


def prep_inputs(inp, core, names=None):
    b, hf = core // 2, core % 2
    f32 = np.float32
    w_in = inp["w_in"][0]
    need = (lambda k: True) if names is None else (lambda k: k in names)
    out = {}
    hall = np.zeros((TK, D), f32)
    hall[:NMETA] = inp["meta_tokens"]
    hall[NMETA:NMETA + SEQ] = inp["x"][b]
    tiles = core_tiles(hf)
    out["hall"] = hall
    out["hq"] = np.concatenate([hall[t * 128:(t + 1) * 128] for t in tiles], axis=0)
    qpos = np.concatenate([np.arange(t * 128, (t + 1) * 128) for t in tiles])
    out["g_attn"] = np.ascontiguousarray(inp["attn_norm_g"][0])
    consts = np.zeros((128, 64), f32)
    consts[:, 0:4] = inp["kv_norm_g"][0].reshape(4, 128).T
    consts[:, 4:12] = inp["q_norm_g"][0].reshape(8, 128).T
    consts[:, 63] = EPS
    out["consts"] = consts
    if need("wk"):
        cols = [w_in[:, OFF["ckv"]:OFF["ckv"] + 512]]
        kpe = w_in[:, OFF["kpe"]:OFF["kpe"] + 64]
        cols += [kpe, swap_halves(kpe, 64)]
        for g in range(4):
            kb = w_in[:, OFF["kb"] + g * 128:OFF["kb"] + (g + 1) * 128]
            cols += [kb, swap_halves(kb, 32)]
        kix = w_in[:, OFF["kix"]:OFF["kix"] + 64]
        cols += [kix, swap_halves(kix, 16)]
        cols.append(w_in[:, OFF["vb"]:OFF["vb"] + 512])
        out["wk"] = np.ascontiguousarray(np.concatenate(cols, axis=1))
        out["ropek"] = rope_tables(np.arange(TK))
    if need("wukv2"):
        w = inp["w_ukv"][0].reshape(512, 16, 2, 128)
        out["wukv2"] = np.ascontiguousarray(np.concatenate([w[:, :, 0, :].reshape(512, 2048), w[:, :, 1, :].reshape(512, 2048)], axis=1))
    if need("wq"):
        cols = [w_in[:, OFF["cq"]:OFF["cq"] + 1024]]
        for h in range(16):
            qb = w_in[:, OFF["qb"] + h * 128:OFF["qb"] + (h + 1) * 128]
            cols += [qb, swap_halves(qb, 32)]
        z48 = np.zeros((D, 48), f32)
        for bk in range(8):
            q0 = w_in[:, OFF["qix"] + (2 * bk) * 64:OFF["qix"] + (2 * bk + 1) * 64]
            q1 = w_in[:, OFF["qix"] + (2 * bk + 1) * 64:OFF["qix"] + (2 * bk + 2) * 64]
            cols += [q0, q1, swap_halves(q0, 16), z48, swap_halves(q1, 16)]
        cols.append(w_in[:, OFF["wix"]:OFF["wix"] + 16])
        cols.append(w_in[:, OFF["ga"]:OFF["ga"] + 4096])
        cols.append(w_in[:, OFF["gb"]:OFF["gb"] + 4096])
        out["wq"] = np.ascontiguousarray(np.concatenate(cols, axis=1))
        out["ropeq"] = rope_tables(qpos)
    if need("wuq2"):
        w = inp["w_uq"][0]
        cols = []
        for h in range(16):
            rp = w[:, h * 192 + 128:(h + 1) * 192]
            cols += [w[:, h * 192:h * 192 + 128], rp, swap_halves(rp, 64)]
        out["wuq2"] = np.ascontiguousarray(np.concatenate(cols, axis=1))
    if need("cmask"):
        cm = np.zeros((NQT, 128, 256), f32)
        for p, t in enumerate(tiles):
            nk = min(2 * p + 2, NKT)
            qp = t * 128 + np.arange(128)
            kp = (nk - 2) * 128 + np.arange(256)
            cm[p] = np.where(kp[None, :] <= qp[:, None], 0.0, NEG)
        out["cmask"] = cm
    if need("wbr"):
        out["wbr"] = inp["w_branch"][0]
    if need("wout"):
        out["wout"] = inp["w_out"][0]
    if need("g_ffn"):
        out["g_ffn"] = np.ascontiguousarray(inp["ffn_norm_g"][0])
    if need("wpq"):
        out["wpq"] = inp["peer_w_q"][0]
    if need("subk"):
        out["subk"] = np.ascontiguousarray(inp["peer_sub_keys"][0].transpose(3, 0, 1, 2).reshape(128, 16, 128))
    if need("peer_u"):
        out["peer_u"] = inp["peer_u"][0]
        out["peer_v"] = inp["peer_v"][0]
    if need("g_fin"):
        out["g_fin"] = np.ascontiguousarray(inp["final_norm_g"])
    return {k: v for k, v in out.items() if need(k)}


def kernel(**inputs):
    inp = {k: np.asarray(v) for k, v in inputs.items()}
    nphase = int(os.environ.get("MK_NPHASE", "99"))
    debug = tuple(x for x in os.environ.get("MK_DEBUG", "").split(",") if x)
    bld = Builder(nphase, debug)
    nc = bld.build()
    names = set(bld.dram.keys())
    in_maps = [prep_inputs(inp, c, names) for c in range(NCORES)]
    res = run_bass_kernel_spmd(nc, in_maps, core_ids=list(range(NCORES)))
    kernel.last = res
    out = np.zeros((4, SEQ, D), np.float32)
    for c in range(NCORES):
        b, hf = c // 2, c % 2
        y = res.results[c]["y"]
        for p, t in enumerate(core_tiles(hf)):
            if 2 * p + hf > 32:
                continue
            lo, hi = t * 128 - NMETA, (t + 1) * 128 - NMETA
            a, bnd = max(lo, 0), min(hi, SEQ)
            out[b, a:bnd] = y[p * 128 + (a - lo):p * 128 + (bnd - lo)]
    return out
```

```python
import os
import types
import numpy as np
import ml_dtypes
import concourse.bass as bass
import concourse.mybir as mybir
from concourse.bass_utils import run_bass_kernel_spmd
from contextlib import ExitStack

F32 = mybir.dt.float32
BF16 = mybir.dt.bfloat16
U32 = mybir.dt.uint32
AF = mybir.ActivationFunctionType
ALU = mybir.AluOpType
AX = mybir.AxisListType

D = 4096
NMETA = 16
SEQ = 4096
TK = 4224
NKT = 33
NQT = 17
TQ = NQT * 128
EPS = 1e-6
NEG = -1.0e30
THETA = 500000.0
NCORES = 8


class Tr:
    __slots__ = ("name", "w", "r", "sem")

    def __init__(self, name):
        self.name = name
        self.w = []
        self.r = []
        self.sem = None


class Op:
    __slots__ = ("eng", "fn", "deps", "signal", "is_dma", "semkey", "val", "epoch")


def _freeze(fn):
    if fn is None or fn.__closure__ is None:
        return fn
    cells = []
    for c in fn.__closure__:
        try:
            cells.append(types.CellType(c.cell_contents))
        except ValueError:
            cells.append(c)
    return types.FunctionType(fn.__code__, fn.__globals__, fn.__name__, fn.__defaults__, tuple(cells))


class Prog:
    ENG = ("pe", "act", "dve", "pool", "sp")

    def __init__(self, nc, es):
        self.nc = nc
        self.es = es
        self.ops = []
        self.epoch = 0
        self.h = {"pe": nc.tensor, "act": nc.scalar, "dve": nc.vector, "pool": nc.gpsimd, "sp": nc.sync}
        self.semobj = {}
        for e in self.ENG:
            self.semobj[e] = es.enter_context(nc.semaphore("sem_" + e))
        self.ndsem = 0
        self.last = {}
        self.free_sems = {}
        self.epoch_trs = []

    def tr(self, name):
        return Tr(name)

    def _dsem(self, t, eng):
        if t.sem is None:
            fl = self.free_sems.setdefault(eng, [])
            if fl:
                key = fl.pop()
            else:
                key = "d%s%d" % (eng, self.ndsem)
                self.ndsem += 1
                self.semobj[key] = self.es.enter_context(self.nc.semaphore("sem_" + key))
            t.sem = key
            self.epoch_trs.append((t, eng))
        return t.sem

    def op(self, eng, fn, reads=(), writes=(), dma=None):
        o = Op()
        o.eng = eng
        o.fn = _freeze(fn)
        o.signal = False
        o.is_dma = dma is not None
        o.semkey = self._dsem(dma, eng) if dma is not None else eng
        o.val = None
        o.epoch = self.epoch
        raw = []
        for t in reads:
            raw.extend(t.w)
        oth = []
        for t in writes:
            oth.extend(t.w)
            oth.extend(t.r)
        deps = {}
        for d, is_raw in [(x, True) for x in raw] + [(x, False) for x in oth]:
            if d.epoch != self.epoch:
                continue
            if not (o.is_dma or d.is_dma) and d.eng == eng:
                if eng == "pe":
                    continue
            deps[id(d)] = d
        o.deps = list(deps.values())
        for d in o.deps:
            d.signal = True
        for t in writes:
            t.w = [o]
            t.r = []
        for t in reads:
            if o.is_dma:
                t.r = [x for x in t.r if x.epoch == self.epoch] + [o]
            else:
                t.r = [x for x in t.r if x.epoch == self.epoch and (x.is_dma or x.eng != eng)] + [o]
        self.ops.append(o)
        self.last[o.semkey] = o
        return o

    def barrier(self):
        lasts = [o for o in self.last.values() if o.epoch == self.epoch]
        for o in lasts:
            o.signal = True
        for e in self.ENG:
            b = Op()
            b.eng = e
            b.fn = None
            b.signal = False
            b.is_dma = False
            b.semkey = e
            b.val = None
            b.epoch = self.epoch
            b.deps = [o for o in lasts if o.is_dma or o.eng != e]
            self.ops.append(b)
        self.epoch += 1
        self.last = {}
        for (t, eng) in self.epoch_trs:
            self.free_sems[eng].append(t.sem)
            t.sem = None
        self.epoch_trs = []

    def emit(self):
        tot = {k: 0 for k in self.semobj}
        known = {e: {} for e in self.ENG}
        nw = 0
        for o in self.ops:
            E = self.h[o.eng]
            need = {}
            for d in o.deps:
                if need.get(d.semkey, 0) < d.val:
                    need[d.semkey] = d.val
            kn = known[o.eng]
            for key, v in need.items():
                if kn.get(key, 0) < v:
                    E.wait_ge(self.semobj[key], v)
                    kn[key] = v
                    nw += 1
            if o.fn is None:
                continue
            ins = o.fn()
            if o.is_dma:
                tot[o.semkey] += 16
                o.val = tot[o.semkey]
                ins.then_inc(self.semobj[o.semkey], 16)
            elif o.signal:
                tot[o.semkey] += 1
                o.val = tot[o.semkey]
                ins.then_inc(self.semobj[o.semkey], 1)
        return nw


class Ring:
    uid = 0

    def __init__(self, P, es, name, shape, dtype, n, psum=False):
        self.t = []
        self.i = 0
        for k in range(n):
            Ring.uid += 1
            nm = "%s%d_%d" % (name, k, Ring.uid)
            if psum:
                tt = es.enter_context(P.nc.psum_tensor(nm, shape, dtype))
            else:
                tt = es.enter_context(P.nc.sbuf_tensor(nm, shape, dtype))
            self.t.append((tt, P.tr(nm)))

    def next(self):
        x = self.t[self.i % len(self.t)]
        self.i += 1
        return x


def groups_of(T, g=512):
    out = []
    t0 = 0
    while t0 < T:
        n = min(g, T - t0)
        out.append((t0, n))
        t0 += n
    return out


class Builder:
    def __init__(self, nphase=99, debug=()):
        self.nphase = nphase
        self.debug = set(debug)
        self.lim = int(os.environ.get("MK_LIM", "0"))
        self.nc = bass.Bass("TRN2", target_bir_lowering=False)
        self.dram = {}

    def un(self, name):
        Ring.uid += 1
        return "%s_%d" % (name, Ring.uid)

    def din(self, name, shape, dtype=F32):
        t = self.nc.dram_tensor(name, list(shape), dtype, kind="ExternalInput")
        self.dram[name] = t
        return t

    def dscr(self, name, shape, dtype=BF16):
        kind = "ExternalOutput" if name in self.debug else "Internal"
        t = self.nc.dram_tensor(name, list(shape), dtype, kind=kind)
        self.dram[name] = t
        return t

    def phase_rownorm_T(self, P, ps, src_list, g_ap, extra=None):
        nc = self.nc
        with ExitStack() as es:
            xin = Ring(P, es, "rn_x", [128, D], F32, 2)
            xad = Ring(P, es, "rn_a", [128, D], F32, 2) if extra else None
            hnr = Ring(P, es, "rn_hn", [128, D], BF16, 2)
            hTr = Ring(P, es, "rn_hT", [128, 32, 128], BF16, 2)
            junk = es.enter_context(nc.sbuf_tensor(self.un("rn_junk"), [128, D], BF16))
            junk_t = P.tr("junk")
            gbc = es.enter_context(nc.sbuf_tensor(self.un("rn_g"), [128, D], F32))
            gbc_t = P.tr("gbc")
            st = Ring(P, es, "rn_st", [128, 4], F32, 4)
            P.op("sp", lambda: nc.sync.dma_start(out=gbc[:], in_=g_ap.partition_broadcast(128)),
                 writes=[gbc_t], dma=gbc_t)
            for (src, dstT, R) in src_list:
                for i in range(min(R // 128, self.lim) if self.lim else R // 128):
                    x, xt = xin.next()
                    P.op("sp", lambda x=x, i=i, src=src: nc.sync.dma_start(out=x[:], in_=src.ap()[i * 128:(i + 1) * 128, :]),
                         writes=[xt], dma=xt)
                    if extra is not None:
                        a, at = xad.next()
                        P.op("sp", lambda a=a, i=i: nc.sync.dma_start(out=a[:], in_=extra[0].ap()[i * 128:(i + 1) * 128, :]),
                             writes=[at], dma=at)
                        P.op("dve", lambda x=x, a=a: nc.vector.tensor_tensor(out=x[:], in0=x[:], in1=a[:], op=ALU.add),
                             reads=[at, xt], writes=[xt])
                        P.op("sp", lambda x=x, i=i: nc.sync.dma_start(out=extra[1].ap()[i * 128:(i + 1) * 128, :], in_=x[:]),
                             reads=[xt], dma=xt)
                    s, s_t = st.next()
                    P.op("act", lambda x=x, s=s: nc.scalar.activation(out=junk[:], in_=x[:], func=AF.Square, accum_out=s[:, 0:1]),
                         reads=[xt], writes=[junk_t, s_t])
                    P.op("act", lambda s=s: nc.scalar.activation(out=s[:, 1:2], in_=s[:, 0:1], func=AF.Sqrt, scale=1.0 / D, bias=self.eps_ap),
                         reads=[s_t, self.const_t], writes=[s_t])
                    P.op("dve", lambda s=s: nc.vector.reciprocal(out=s[:, 2:3], in_=s[:, 1:2]), reads=[s_t], writes=[s_t])
                    hn, hnt = hnr.next()
                    P.op("dve", lambda x=x, s=s, hn=hn: nc.vector.scalar_tensor_tensor(
                        out=hn[:], in0=x[:], scalar=s[:, 2:3], op0=ALU.mult, in1=gbc[:], op1=ALU.mult),
                        reads=[xt, s_t, gbc_t], writes=[hnt])
                    hT, hTt = hTr.next()
                    for cb in range(8):
                        pt, ptt = ps.next()
                        for c in range(4):
                            cc = cb * 4 + c
                            P.op("pe", lambda pt=pt, hn=hn, c=c, cc=cc: nc.tensor.matmul(
                                pt[:, c * 128:(c + 1) * 128], lhsT=hn[:, cc * 128:(cc + 1) * 128], rhs=self.ident[:],
                                start=True, stop=True), reads=[hnt, self.const_t], writes=[ptt])
                        dst = hT[:, cb * 4:(cb + 1) * 4, :]
                        srcp = pt[:, :].rearrange("p (c t) -> p c t", c=4)
                        if cb % 2 == 0:
                            P.op("act", lambda dst=dst, srcp=srcp: nc.scalar.copy(out=dst, in_=srcp), reads=[ptt], writes=[hTt])
                        else:
                            P.op("dve", lambda dst=dst, srcp=srcp: nc.vector.tensor_copy(out=dst, in_=srcp), reads=[ptt], writes=[hTt])
                    P.op("sp", lambda hT=hT, i=i, dstT=dstT: nc.sync.dma_start(
                        out=dstT.ap()[:, :, i * 128:(i + 1) * 128].rearrange("k p t -> p k t"), in_=hT[:]),
                        reads=[hTt], dma=hTt)
        P.barrier()

    def phase_gemm(self, P, ps, Wd, Kdim, XT, T, units, rope=None, gates=None):
        nc = self.nc
        KC = Kdim // 128
        blocks = []
        cur = []
        c_start = None
        for u in units:
            w = u["m"] + u.get("mb", 0)
            if cur and (u.get("brk") or u["c0"] + w - c_start > 512 or u["kind"] == "tok" or cur[-1]["kind"] == "tok" or (u["kind"] == "branch" and u["c0"] + w - c_start > 512)
                        or u["c0"] != cur[-1]["c0"] + cur[-1]["m"] + cur[-1].get("mb", 0)):
                blocks.append((c_start, cur))
                cur = []
            if not cur:
                c_start = u["c0"]
            cur.append(u)
        if cur:
            blocks.append((c_start, cur))
        tg = groups_of(T)
        if self.lim:
            tg = tg[:max(1, self.lim // 4)]
        with ExitStack() as es:
            wr = Ring(P, es, "g_w", [128, KC, 512], BF16, 2)
            xr = Ring(P, es, "g_x", [128, KC, 512], BF16, 2)
            sr = Ring(P, es, "g_s", [128, 512], F32, 4)
            tr_ = Ring(P, es, "g_t", [128, 512], F32, 3)
            rr = Ring(P, es, "g_r", [128, 6, 512], F32, 2) if rope is not None else None
            gr = Ring(P, es, "g_g", [128, 512], BF16, 4) if gates is not None else None
            for (c_start, ulist) in blocks:
                last = ulist[-1]
                bw = last["c0"] + last["m"] + last.get("mb", 0) - c_start
                w, wt = wr.next()
                half = KC // 2 if KC >= 2 else KC
                for k0 in range(0, KC, half):
                    P.op("pool", lambda w=w, k0=k0, c_start=c_start, bw=bw, half=half: nc.gpsimd.dma_start(
                        out=w[:, k0:k0 + half, :bw],
                        in_=Wd.ap()[k0 * 128:(k0 + half) * 128, c_start:c_start + bw].rearrange("(k p) c -> p k c", p=128)),
                        writes=[wt], dma=wt)
                for (t0, n) in tg:
                    x, xt = xr.next()
                    P.op("sp", lambda x=x, t0=t0, n=n: nc.sync.dma_start(
                        out=x[:, :, :n], in_=XT.ap()[:, :, t0:t0 + n].rearrange("k p t -> p k t")),
                        writes=[xt], dma=xt)
                    if rope is not None and any(u["kind"] == "rope" for u in ulist):
                        rt, rtt = rr.next()
                        P.op("sp", lambda rt=rt, t0=t0, n=n: nc.sync.dma_start(
                            out=rt[:, :, :n], in_=rope.ap()[:, :, t0:t0 + n].rearrange("k p t -> p k t")),
                            writes=[rtt], dma=rtt)
                    for u in ulist:
                        kind = u["kind"]
                        off = u["c0"] - c_start
                        m = u["m"]
                        if kind == "tok":
                            for tt in range(n // 128):
                                pt, ptt = ps.next()
                                for kc in range(KC):
                                    P.op("pe", lambda pt=pt, x=x, w=w, kc=kc, tt=tt, off=off, m=m: nc.tensor.matmul(
                                        pt[:, :m], lhsT=x[:, kc, tt * 128:(tt + 1) * 128], rhs=w[:, kc, off:off + m],
                                        start=(kc == 0), stop=(kc == KC - 1)), reads=[xt, wt], writes=[ptt])
                                s, s_t = sr.next()
                                sv = s[:, :m] if u["dtype"] == F32 else s[:, :].bitcast(BF16)[:, :m]
                                r0 = t0 + tt * 128
                                if u.get("res") is not None:
                                    rs_, rst = tr_.next()
                                    P.op("sp", lambda rs_=rs_, u=u, r0=r0, m=m: nc.sync.dma_start(
                                        out=rs_[:, :m], in_=u["res"].ap()[r0:r0 + 128, u["col"]:u["col"] + m]), writes=[rst], dma=rst)
                                    P.op("dve", lambda sv=sv, pt=pt, rs_=rs_, m=m: nc.vector.tensor_tensor(out=sv, in0=pt[:, :m], in1=rs_[:, :m], op=ALU.add),
                                         reads=[ptt, rst], writes=[s_t])
                                else:
                                    P.op("act", lambda sv=sv, pt=pt, m=m: nc.scalar.copy(out=sv, in_=pt[:, :m]), reads=[ptt], writes=[s_t])
                                P.op("sp", lambda sv=sv, u=u, r0=r0, m=m: nc.sync.dma_start(
                                    out=u["dst"].ap()[r0:r0 + 128, u["col"]:u["col"] + m], in_=sv), reads=[s_t], dma=s_t)
                            continue
                        if kind == "branch":
                            pa_, pat = ps.next()
                            pb_, pbt = ps.next()
                            hk = KC // 2
                            for kc in range(KC):
                                tgt, tgtt = (pa_, pat) if kc < hk else (pb_, pbt)
                                P.op("pe", lambda tgt=tgt, x=x, w=w, kc=kc, off=off, n=n, hk=hk: nc.tensor.matmul(
                                    tgt[:, :n], lhsT=w[:, kc, off:off + 128], rhs=x[:, kc, :n],
                                    start=(kc % hk == 0), stop=(kc % hk == hk - 1)), reads=[xt, wt], writes=[tgtt])
                            g1, g1t = gr.next()
                            g2, g2t = gr.next()
                            P.op("sp", lambda g1=g1, u=u, t0=t0, n=n: nc.sync.dma_start(out=g1[:, :n], in_=gates[0].ap()[u["idx"], :, t0:t0 + n]),
                                 writes=[g1t], dma=g1t)
                            P.op("sp", lambda g2=g2, u=u, t0=t0, n=n: nc.sync.dma_start(out=g2[:, :n], in_=gates[1].ap()[u["idx"], :, t0:t0 + n]),
                                 writes=[g2t], dma=g2t)
                            t1, t1t = tr_.next()
                            s, s_t = sr.next()
                            sb = s[:, :].bitcast(BF16)
                            P.op("dve", lambda t1=t1, pa_=pa_, g1=g1, n=n: nc.vector.tensor_tensor(out=t1[:, :n], in0=pa_[:, :n], in1=g1[:, :n], op=ALU.mult),
                                 reads=[pat, g1t], writes=[t1t])
                            t2, t2t = tr_.next()
                            P.op("dve", lambda t2=t2, pb_=pb_, g2=g2, n=n: nc.vector.tensor_tensor(out=t2[:, :n], in0=pb_[:, :n], in1=g2[:, :n], op=ALU.mult),
                                 reads=[pbt, g2t], writes=[t2t])
                            P.op("dve", lambda sb=sb, t1=t1, t2=t2, n=n: nc.vector.tensor_tensor(out=sb[:, :n], in0=t1[:, :n], in1=t2[:, :n], op=ALU.add),
                                 reads=[t1t, t2t], writes=[s_t])
                            P.op("sp", lambda sb=sb, u=u, t0=t0, n=n: nc.sync.dma_start(
                                out=u["dst"].ap()[u["idx"], :, t0:t0 + n], in_=sb[:, :n]), reads=[s_t], dma=s_t)
                            continue
                        pt, ptt = ps.next()
                        for kc in range(KC):
                            P.op("pe", lambda pt=pt, x=x, w=w, kc=kc, off=off, m=m, n=n: nc.tensor.matmul(
                                pt[:m, :n], lhsT=w[:, kc, off:off + m], rhs=x[:, kc, :n],
                                start=(kc == 0), stop=(kc == KC - 1)), reads=[xt, wt], writes=[ptt])
                        s, s_t = sr.next()
                        sb = s[:, :].bitcast(BF16)
                        if kind == "plain":
                            P.op("act", lambda sb=sb, pt=pt, m=m, n=n: nc.scalar.copy(out=sb[:m, :n], in_=pt[:m, :n]), reads=[ptt], writes=[s_t])
                        elif kind == "sig":
                            P.op("act", lambda sb=sb, pt=pt, m=m, n=n: nc.scalar.activation(out=sb[:m, :n], in_=pt[:m, :n], func=AF.Sigmoid),
                                 reads=[ptt], writes=[s_t])
                        elif kind == "rope":
                            mb = u["mb"]
                            pb, pbt = ps.next()
                            for kc in range(KC):
                                P.op("pe", lambda pb=pb, x=x, w=w, kc=kc, off=off, m=m, mb=mb, n=n: nc.tensor.matmul(
                                    pb[:mb, :n], lhsT=w[:, kc, off + m:off + m + mb], rhs=x[:, kc, :n],
                                    start=(kc == 0), stop=(kc == KC - 1)), reads=[xt, wt], writes=[pbt])
                            P.op("act", lambda sb=sb, pt=pt, m=m, n=n: nc.scalar.copy(out=sb[:m, :n], in_=pt[:m, :n]), reads=[ptt], writes=[s_t])
                            ti = u["ti"]
                            for (r0, rc) in (u["rows"] if not os.environ.get("MK_NOROPE") else []):
                                t1, t1t = tr_.next()
                                t2, t2t = tr_.next()
                                P.op("act", lambda t1=t1, pt=pt, r0=r0, rc=rc, n=n: nc.scalar.copy(out=t1[r0:r0 + rc, :n], in_=pt[r0:r0 + rc, :n]),
                                     reads=[ptt], writes=[t1t])
                                P.op("act", lambda t2=t2, pb=pb, r0=r0, rc=rc, n=n: nc.scalar.copy(out=t2[r0:r0 + rc, :n], in_=pb[r0:r0 + rc, :n]),
                                     reads=[pbt], writes=[t2t])
                                P.op("dve", lambda t1=t1, rt=rt, r0=r0, rc=rc, n=n, ti=ti: nc.vector.tensor_tensor(
                                    out=t1[r0:r0 + rc, :n], in0=t1[r0:r0 + rc, :n], in1=rt[r0:r0 + rc, 2 * ti, :n], op=ALU.mult),
                                    reads=[t1t, rtt], writes=[t1t])
                                P.op("dve", lambda t2=t2, rt=rt, r0=r0, rc=rc, n=n, ti=ti: nc.vector.tensor_tensor(
                                    out=t2[r0:r0 + rc, :n], in0=t2[r0:r0 + rc, :n], in1=rt[r0:r0 + rc, 2 * ti + 1, :n], op=ALU.mult),
                                    reads=[t2t, rtt], writes=[t2t])
                                P.op("dve", lambda sb=sb, t1=t1, t2=t2, r0=r0, rc=rc, n=n: nc.vector.tensor_tensor(
                                    out=sb[r0:r0 + rc, :n], in0=t1[r0:r0 + rc, :n], in1=t2[r0:r0 + rc, :n], op=ALU.add),
                                    reads=[t1t, t2t], writes=[s_t])
                        P.op("sp", lambda sb=sb, u=u, t0=t0, m=m, n=n: nc.sync.dma_start(
                            out=u["dst"].ap()[u["idx"], :m, t0:t0 + n], in_=sb[:m, :n]), reads=[s_t], dma=s_t)
        P.barrier()

    def phase_fnorm(self, P, ps, jobs):
        nc = self.nc
        with ExitStack() as es:
            xr = Ring(P, es, "fn_x", [128, 8, 512], BF16, 2)
            qr = Ring(P, es, "fn_q", [128, 8, 512], BF16, 2)
            orr = Ring(P, es, "fn_o", [128, 8, 512], BF16, 2)
            rs = Ring(P, es, "fn_r", [128, 2, 512], F32, 2)
            for (src, dst, nsub, T, gcol) in jobs:
                nf = nsub * 128
                for (t0, n) in (groups_of(T)[:max(1, self.lim // 4)] if self.lim else groups_of(T)):
                    x, xt = xr.next()
                    P.op("sp", lambda x=x, src=src, nsub=nsub, t0=t0, n=n: nc.sync.dma_start(
                        out=x[:, :nsub, :n], in_=src.ap()[:, :, t0:t0 + n].rearrange("k p t -> p k t")), writes=[xt], dma=xt)
                    q, qt = qr.next()
                    P.op("act", lambda q=q, x=x, nsub=nsub, n=n: nc.scalar.activation(out=q[:, :nsub, :n], in_=x[:, :nsub, :n], func=AF.Square),
                         reads=[xt], writes=[qt])
                    pt, ptt = ps.next()
                    for i in range(nsub):
                        P.op("pe", lambda pt=pt, q=q, i=i, n=n: nc.tensor.matmul(pt[:, :n], lhsT=self.ones[:], rhs=q[:, i, :n],
                                                                                 start=(i == 0), stop=(i == nsub - 1)),
                             reads=[qt, self.const_t], writes=[ptt])
                    r, rt = rs.next()
                    P.op("act", lambda r=r, pt=pt, n=n, nf=nf: nc.scalar.activation(out=r[:, 0, :n], in_=pt[:, :n], func=AF.Sqrt, scale=1.0 / nf, bias=self.eps_ap),
                         reads=[ptt, self.const_t], writes=[rt])
                    P.op("dve", lambda r=r, n=n: nc.vector.reciprocal(out=r[:, 1, :n], in_=r[:, 0, :n]), reads=[rt], writes=[rt])
                    o, ot = orr.next()
                    for i in range(nsub):
                        P.op("dve", lambda o=o, x=x, r=r, i=i, n=n, gcol=gcol: nc.vector.scalar_tensor_tensor(
                            out=o[:, i, :n], in0=x[:, i, :n], scalar=gcol[:, i:i + 1], op0=ALU.mult, in1=r[:, 1, :n], op1=ALU.mult),
                            reads=[xt, rt, self.const_t], writes=[ot])
                    P.op("sp", lambda o=o, dst=dst, nsub=nsub, t0=t0, n=n: nc.sync.dma_start(
                        out=dst.ap()[:, :, t0:t0 + n].rearrange("k p t -> p k t"), in_=o[:, :nsub, :n]), reads=[ot], dma=ot)
        P.barrier()

    def softmax_pv(self, P, ps, R, S, St, nk, vfn, out_ap, out_t):
        nc = self.nc
        ncol = nk * 128
        s, s_t = R["st"].next()
        P.op("dve", lambda: nc.vector.tensor_reduce(out=s[:, 0:1], in_=S[:, :ncol], op=ALU.max, axis=AX.X, negate=True),
             reads=[St], writes=[s_t])
        pb, pbt = R["pb"].next()
        P.op("act", lambda: nc.scalar.activation(out=pb[:, :ncol], in_=S[:, :ncol], func=AF.Exp, bias=s[:, 0:1], scale=1.0,
                                                 accum_out=s[:, 1:2]), reads=[St, s_t], writes=[pbt, s_t])
        P.op("dve", lambda: nc.vector.reciprocal(out=s[:, 2:3], in_=s[:, 1:2]), reads=[s_t], writes=[s_t])
        dg, dgt = R["dg"].next()
        P.op("dve", lambda: nc.vector.tensor_scalar(out=dg[:], in0=self.ident[:], scalar1=s[:, 2:3], scalar2=None, op0=ALU.mult),
             reads=[s_t, self.const_t], writes=[dgt])
        PT, PTt = R["pt"].next()
        for kq in range((nk + 3) // 4):
            cnt = min(4, nk - 4 * kq)
            pt, ptt = ps.next()
            for j in range(cnt):
                kt = 4 * kq + j
                P.op("pe", lambda pt=pt, j=j, kt=kt: nc.tensor.matmul(pt[:, j * 128:(j + 1) * 128], lhsT=pb[:, kt * 128:(kt + 1) * 128],
                                                                      rhs=dg[:], start=True, stop=True), reads=[pbt, dgt], writes=[ptt])
            dst = PT[:, 4 * kq:4 * kq + cnt, :]
            src = pt[:, :cnt * 128].rearrange("p (c t) -> p c t", c=cnt)
            if kq % 2 == 0:
                P.op("act", lambda dst=dst, src=src: nc.scalar.copy(out=dst, in_=src), reads=[ptt], writes=[PTt])
            else:
                P.op("dve", lambda dst=dst, src=src: nc.vector.tensor_copy(out=dst, in_=src), reads=[ptt], writes=[PTt])
        po, pot = ps.next()
        for kt in range(nk):
            v_ap, v_t = vfn(kt)
            P.op("pe", lambda kt=kt, v_ap=v_ap: nc.tensor.matmul(po[:, :128], lhsT=v_ap, rhs=PT[:, kt, :], start=(kt == 0), stop=(kt == nk - 1)),
                 reads=[v_t, PTt], writes=[pot])
        P.op("act", lambda: nc.scalar.copy(out=out_ap, in_=po[:, :128]), reads=[pot], writes=[out_t])

    def nq_loop(self):
        return range(min(NQT, self.lim) if self.lim else NQT)

    def phase_mla(self, P, ps, kanT, kpeT, va, qanT, qarT, cmask, oT, oT_base):
        nc = self.nc
        scale = 192.0 ** -0.5
        with ExitStack() as es:
            kn = Ring(P, es, "ml_kn", [128, TK], BF16, 2)
            vh = Ring(P, es, "ml_v", [128, NKT, 128], BF16, 2)
            qn = Ring(P, es, "ml_qn", [128, TQ], BF16, 2)
            qr = Ring(P, es, "ml_qr", [64, TQ], BF16, 2)
            oR = Ring(P, es, "ml_o", [128, TQ], BF16, 2)
            R = dict(st=Ring(P, es, "ml_st", [128, 4], F32, 4), pb=Ring(P, es, "ml_pb", [128, TK], BF16, 2),
                     dg=Ring(P, es, "ml_dg", [128, 128], BF16, 2), pt=Ring(P, es, "ml_pt", [128, NKT, 128], BF16, 2))
            Sr = Ring(P, es, "ml_s", [128, TK], F32, 2)
            kpe = es.enter_context(nc.sbuf_tensor(self.un("ml_kpe"), [64, TK], BF16))
            cm = es.enter_context(nc.sbuf_tensor(self.un("ml_cm"), [128, NQT, 256], F32))
            ct = P.tr("ml_c")
            ct2 = P.tr("ml_c2")
            P.op("sp", lambda: nc.sync.dma_start(out=kpe[:], in_=kpeT.ap()[0]), writes=[ct], dma=ct)
            P.op("sp", lambda: nc.sync.dma_start(out=cm[:], in_=cmask.ap().rearrange("q p c -> p q c")), writes=[ct2], dma=ct2)
            for h in range(16):
                k_, kt_ = kn.next()
                v_, vt_ = vh.next()
                qn_, qnt = qn.next()
                qr_, qrt = qr.next()
                P.op("sp", lambda k_=k_, h=h: nc.sync.dma_start(out=k_[:], in_=kanT.ap()[h]), writes=[kt_], dma=kt_)
                P.op("sp", lambda v_=v_, h=h: nc.sync.dma_start(out=v_[:], in_=va.ap()[:, h * 128:(h + 1) * 128].rearrange("(k p) d -> p k d", p=128)),
                     writes=[vt_], dma=vt_)
                P.op("sp", lambda qn_=qn_, h=h: nc.sync.dma_start(out=qn_[:], in_=qanT.ap()[h]), writes=[qnt], dma=qnt)
                P.op("sp", lambda qr_=qr_, h=h: nc.sync.dma_start(out=qr_[:], in_=qarT.ap()[h]), writes=[qrt], dma=qrt)
                o, ot = oR.next()
                for p in self.nq_loop():
                    nk = min(2 * p + 2, NKT)
                    ncol = nk * 128
                    S, St = Sr.next()
                    for (c0, n) in groups_of(ncol):
                        pt, ptt = ps.next()
                        P.op("pe", lambda pt=pt, p=p, c0=c0, n=n: nc.tensor.matmul(pt[:, :n], lhsT=qn_[:, p * 128:(p + 1) * 128], rhs=k_[:, c0:c0 + n],
                                                                                  start=True, stop=False), reads=[qnt, kt_], writes=[ptt])
                        P.op("pe", lambda pt=pt, p=p, c0=c0, n=n: nc.tensor.matmul(pt[:, :n], lhsT=qr_[:, p * 128:(p + 1) * 128], rhs=kpe[:, c0:c0 + n],
                                                                                  start=False, stop=True), reads=[qrt, ct], writes=[ptt])
                        P.op("act", lambda pt=pt, S=S, c0=c0, n=n: nc.scalar.mul(out=S[:, c0:c0 + n], in_=pt[:, :n], mul=scale), reads=[ptt], writes=[St])
                    P.op("dve", lambda S=S, p=p, ncol=ncol: nc.vector.tensor_tensor(out=S[:, ncol - 256:ncol], in0=S[:, ncol - 256:ncol], in1=cm[:, p, :], op=ALU.add),
                         reads=[St, ct2], writes=[St])
                    self.softmax_pv(P, ps, R, S, St, nk, lambda kt, v_=v_, vt_=vt_: (v_[:, kt, :], vt_), o[:, p * 128:(p + 1) * 128], ot)
                nqc = len(self.nq_loop()) * 128
                P.op("sp", lambda o=o, h=h: nc.sync.dma_start(out=oT.ap()[oT_base + h, :, :nqc], in_=o[:, :nqc]), reads=[ot], dma=ot)
        P.barrier()

    def phase_dsa(self, P, ps, kbT, vb, kixT, qbT, qixT, wix, cmask, oT, oT_base, topk=256):
        nc = self.nc
        scale = 128.0 ** -0.5
        with ExitStack() as es:
            kb = es.enter_context(nc.sbuf_tensor(self.un("ds_kb"), [128, 4, TK], BF16))
            vv = es.enter_context(nc.sbuf_tensor(self.un("ds_v"), [128, NKT, 512], BF16))
            kx = es.enter_context(nc.sbuf_tensor(self.un("ds_kx"), [128, TK], BF16))
            cm = es.enter_context(nc.sbuf_tensor(self.un("ds_cm"), [128, NQT, 256], F32))
            ct = [P.tr("ds_c%d" % i) for i in range(5)]
            P.op("sp", lambda: nc.sync.dma_start(out=kb[:], in_=kbT.ap().rearrange("g p t -> p g t")), writes=[ct[0]], dma=ct[0])
            P.op("sp", lambda: nc.sync.dma_start(out=vv[:], in_=vb.ap().rearrange("(k p) d -> p k d", p=128)), writes=[ct[1]], dma=ct[1])
            P.op("sp", lambda: nc.sync.dma_start(out=kx[0:64, :], in_=kixT.ap()[0]), writes=[ct[2]], dma=ct[2])
            P.op("sp", lambda: nc.sync.dma_start(out=kx[64:128, :], in_=kixT.ap()[0]), writes=[ct[3]], dma=ct[3])
            P.op("sp", lambda: nc.sync.dma_start(out=cm[:], in_=cmask.ap().rearrange("q p c -> p q c")), writes=[ct[4]], dma=ct[4])
            qxr = Ring(P, es, "ds_qx", [128, 8, 128], BF16, 2)
            qbr = Ring(P, es, "ds_qb", [128, 16, 128], BF16, 2)
            wr = Ring(P, es, "ds_w", [128, 16], F32, 2)
            rr = Ring(P, es, "ds_r", [128, 512], F32, 3)
            acc = es.enter_context(nc.sbuf_tensor(self.un("ds_acc"), [128, TK], F32))
            acct = P.tr("ds_acc")
            W0 = es.enter_context(nc.sbuf_tensor(self.un("ds_w0"), [128, TK], F32))
            W0t = P.tr("ds_w0")
            W1 = es.enter_context(nc.sbuf_tensor(self.un("ds_w1"), [128, TK], F32))
            W1t = P.tr("ds_w1")
            m8r = Ring(P, es, "ds_m8", [128, 8], F32, 4)
            thr = Ring(P, es, "ds_thr", [128, 2], F32, 2)
            oR = Ring(P, es, "ds_o", [128, 16, 128], BF16, 2)
            R = dict(st=Ring(P, es, "ds_st", [128, 4], F32, 4), pb=Ring(P, es, "ds_pb", [128, TK], BF16, 1),
                     dg=Ring(P, es, "ds_dg", [128, 128], BF16, 2), pt=Ring(P, es, "ds_pt", [128, NKT, 128], BF16, 1))
            for p in self.nq_loop():
                nk = min(2 * p + 2, NKT)
                ncol = nk * 128
                qx, qxt = qxr.next()
                qb, qbt = qbr.next()
                w_, wt_ = wr.next()
                P.op("sp", lambda qx=qx, p=p: nc.sync.dma_start(out=qx[:], in_=qixT.ap()[:, :, p * 128:(p + 1) * 128].rearrange("k p t -> p k t")),
                     writes=[qxt], dma=qxt)
                P.op("sp", lambda qb=qb, p=p: nc.sync.dma_start(out=qb[:], in_=qbT.ap()[:, :, p * 128:(p + 1) * 128].rearrange("k p t -> p k t")),
                     writes=[qbt], dma=qbt)
                P.op("sp", lambda w_=w_, p=p: nc.sync.dma_start(out=w_[:], in_=wix.ap()[p * 128:(p + 1) * 128, :]), writes=[wt_], dma=wt_)
                for hi in range(16):
                    b, r0 = hi // 2, (hi % 2) * 64
                    for (c0, n) in groups_of(ncol):
                        pt, ptt = ps.next()
                        P.op("pe", lambda pt=pt, qx=qx, b=b, r0=r0, c0=c0, n=n: nc.tensor.matmul(
                            pt[:, :n], lhsT=qx[r0:r0 + 64, b, :], rhs=kx[r0:r0 + 64, c0:c0 + n], start=True, stop=True),
                            reads=[qxt, ct[2], ct[3]], writes=[ptt])
                        r, rt = rr.next()
                        P.op("act", lambda r=r, pt=pt, n=n: nc.scalar.activation(out=r[:, :n], in_=pt[:, :n], func=AF.Relu), reads=[ptt], writes=[rt])
                        if hi == 0:
                            P.op("dve", lambda r=r, w_=w_, c0=c0, n=n, hi=hi: nc.vector.tensor_scalar(
                                out=acc[:, c0:c0 + n], in0=r[:, :n], scalar1=w_[:, hi:hi + 1], scalar2=None, op0=ALU.mult),
                                reads=[rt, wt_], writes=[acct])
                        else:
                            P.op("dve", lambda r=r, w_=w_, c0=c0, n=n, hi=hi: nc.vector.scalar_tensor_tensor(
                                out=acc[:, c0:c0 + n], in0=r[:, :n], scalar=w_[:, hi:hi + 1], op0=ALU.mult, in1=acc[:, c0:c0 + n], op1=ALU.add),
                                reads=[rt, wt_, acct], writes=[acct])
                P.op("dve", lambda p=p, ncol=ncol: nc.vector.tensor_tensor(out=acc[:, ncol - 256:ncol], in0=acc[:, ncol - 256:ncol], in1=cm[:, p, :], op=ALU.add),
                     reads=[acct, ct[4]], writes=[acct])
                th, tht = thr.next()
                if ncol > topk:
                    cur, curt = acc, acct
                    bufs = [(W0, W0t), (W1, W1t)]
                    nr = topk // 8
                    for r_i in range(nr):
                        m8, m8t = m8r.next()
                        P.op("dve", lambda m8=m8, cur=cur, ncol=ncol: nc.vector.max(out=m8[:], in_=cur[:, :ncol]), reads=[curt], writes=[m8t])
                        if r_i == nr - 1:
                            P.op("dve", lambda m8=m8, th=th: nc.vector.tensor_scalar(out=th[:, 0:1], in0=m8[:, 7:8], scalar1=-1.0e29, scalar2=None, op0=ALU.max),
                                 reads=[m8t], writes=[tht])
                            break
                        nxt, nxtt = bufs[r_i % 2]
                        P.op("dve", lambda m8=m8, cur=cur, nxt=nxt, ncol=ncol: nc.vector.match_replace(
                            out=nxt[:, :ncol], in_to_replace=m8[:], in_values=cur[:, :ncol], imm_value=NEG), reads=[curt, m8t], writes=[nxtt])
                        cur, curt = nxt, nxtt
                else:
                    P.op("dve", lambda th=th: nc.vector.memset(th[:, 0:1], -1.0e29), writes=[tht])
                P.op("dve", lambda th=th, ncol=ncol: nc.vector.tensor_scalar(out=W1[:, :ncol], in0=acc[:, :ncol], scalar1=th[:, 0:1], scalar2=NEG,
                                                                             op0=ALU.is_lt, op1=ALU.mult), reads=[acct, tht], writes=[W1t])
                o, ot = oR.next()
                for h in range(16):
                    g = h // 4
                    S, St = W0, W0t
                    for (c0, n) in groups_of(ncol):
                        pt, ptt = ps.next()
                        P.op("pe", lambda pt=pt, qb=qb, h=h, g=g, c0=c0, n=n: nc.tensor.matmul(pt[:, :n], lhsT=qb[:, h, :], rhs=kb[:, g, c0:c0 + n],
                                                                                          start=True, stop=True), reads=[qbt, ct[0]], writes=[ptt])
                        P.op("dve", lambda pt=pt, c0=c0, n=n: nc.vector.scalar_tensor_tensor(out=W0[:, c0:c0 + n], in0=pt[:, :n], scalar=scale, op0=ALU.mult,
                                                                                              in1=W1[:, c0:c0 + n], op1=ALU.add), reads=[ptt, W1t], writes=[W0t])
                    self.softmax_pv(P, ps, R, S, St, nk, lambda kt, g=g: (vv[:, kt, g * 128:(g + 1) * 128], ct[1]), o[:, h, :], ot)
                P.op("sp", lambda o=o, p=p: nc.sync.dma_start(out=oT.ap()[oT_base:oT_base + 16, :, p * 128:(p + 1) * 128].rearrange("k p t -> p k t"), in_=o[:]),
                     reads=[ot], dma=ot)
        P.barrier()

    def phase_peer_gates(self, P, ps, pqT, subk, gT):
        nc = self.nc
        with ExitStack() as es:
            keys = es.enter_context(nc.sbuf_tensor(self.un("pg_k"), [128, 16, 128], BF16))
            kt_ = P.tr("pg_k")
            P.op("pool", lambda: nc.gpsimd.dma_start(out=keys[:], in_=subk.ap()), writes=[kt_], dma=kt_)
            pqr = Ring(P, es, "pg_q", [128, 16, 128], BF16, 2)
            sr = Ring(P, es, "pg_s", [128, 16, 128], F32, 2)
            wr = Ring(P, es, "pg_w", [128, 16, 128], F32, 1)
            m8r = Ring(P, es, "pg_m8", [128, 16, 16], F32, 2)
            cr = Ring(P, es, "pg_c", [128, 8, 256], F32, 1)
            cwr = Ring(P, es, "pg_cw", [128, 8, 256], F32, 1)
            c8r = Ring(P, es, "pg_c8", [128, 8, 24], F32, 2)
            smr = Ring(P, es, "pg_sm", [128, 8, 8], F32, 2)
            e16r = Ring(P, es, "pg_e16", [128, 8, 16], F32, 2)
            nAr = Ring(P, es, "pg_nA", [128, 8, 128], F32, 2)
            E1r = Ring(P, es, "pg_E1", [128, 8, 128], BF16, 2)
            E2r = Ring(P, es, "pg_E2", [128, 8, 128], BF16, 2)
            tmr = Ring(P, es, "pg_tm", [128, 8, 128], F32, 2)
            mkr = Ring(P, es, "pg_mk", [128, 8, 128], BF16, 3)
            Mr = Ring(P, es, "pg_M", [128, 8, 128], BF16, 3)
            gsr = Ring(P, es, "pg_gs", [128, 4, 128], BF16, 3)
            for tl in self.nq_loop():
                pq, pqt = pqr.next()
                P.op("sp", lambda pq=pq, tl=tl: nc.sync.dma_start(out=pq[:], in_=pqT.ap()[:, :, tl * 128:(tl + 1) * 128].rearrange("k p t -> p k t")),
                     writes=[pqt], dma=pqt)
                s, st = sr.next()
                for q4 in range(4):
                    pt, ptt = ps.next()
                    for j in range(4):
                        hc = q4 * 4 + j
                        P.op("pe", lambda pt=pt, pq=pq, hc=hc, j=j: nc.tensor.matmul(pt[:, j * 128:(j + 1) * 128], lhsT=pq[:, hc, :], rhs=keys[:, hc, :],
                                                                                    start=True, stop=True), reads=[pqt, kt_], writes=[ptt])
                    P.op("act", lambda pt=pt, s=s, q4=q4: nc.scalar.copy(out=s[:, q4 * 4:(q4 + 1) * 4, :], in_=pt[:, :].rearrange("p (c n) -> p c n", c=4)),
                         reads=[ptt], writes=[st])
                w, wt = wr.next()
                m8, m8t = m8r.next()
                for hc in range(16):
                    P.op("dve", lambda m8=m8, s=s, hc=hc: nc.vector.max(out=m8[:, hc, 0:8], in_=s[:, hc, :]), reads=[st], writes=[m8t])
                    P.op("dve", lambda m8=m8, s=s, w=w, hc=hc: nc.vector.match_replace(out=w[:, hc, :], in_to_replace=m8[:, hc, 0:8], in_values=s[:, hc, :],
                                                                                      imm_value=NEG), reads=[st, m8t], writes=[wt])
                    P.op("dve", lambda m8=m8, w=w, hc=hc: nc.vector.max(out=m8[:, hc, 8:16], in_=w[:, hc, :]), reads=[wt], writes=[m8t])
                c, ct = cr.next()
                for h in range(8):
                    P.op("dve", lambda c=c, m8=m8, h=h: nc.vector.tensor_tensor(
                        out=c[:, h, :].rearrange("p (a b) -> p a b", a=16),
                        in0=m8[:, 2 * h, :].unsqueeze(2).to_broadcast([128, 16, 16]),
                        in1=m8[:, 2 * h + 1, :].unsqueeze(1).to_broadcast([128, 16, 16]), op=ALU.add), reads=[m8t], writes=[ct])
                cw, cwt = cwr.next()
                c8, c8t = c8r.next()
                for h in range(8):
                    P.op("dve", lambda c8=c8, c=c, h=h: nc.vector.max(out=c8[:, h, 0:8], in_=c[:, h, :]), reads=[ct], writes=[c8t])
                    P.op("dve", lambda c8=c8, c=c, cw=cw, h=h: nc.vector.match_replace(out=cw[:, h, :], in_to_replace=c8[:, h, 0:8], in_values=c[:, h, :],
                                                                                      imm_value=NEG), reads=[ct, c8t], writes=[cwt])
                    P.op("dve", lambda c8=c8, cw=cw, h=h: nc.vector.max(out=c8[:, h, 8:16], in_=cw[:, h, :]), reads=[cwt], writes=[c8t])
                    P.op("dve", lambda c8=c8, cw=cw, c=c, h=h: nc.vector.match_replace(out=c[:, h, :], in_to_replace=c8[:, h, 8:16], in_values=cw[:, h, :],
                                                                                      imm_value=NEG), reads=[cwt, c8t], writes=[ct])
                    P.op("dve", lambda c8=c8, c=c, h=h: nc.vector.max(out=c8[:, h, 16:24], in_=c[:, h, :]), reads=[ct], writes=[c8t])
                sm, smt = smr.next()
                P.op("dve", lambda sm=sm, c8=c8: nc.vector.tensor_tensor(out=sm[:, :, 0:1], in0=c8[:, :, 15:16], in1=c8[:, :, 16:17], op=ALU.add),
                     reads=[c8t], writes=[smt])
                P.op("dve", lambda sm=sm: nc.vector.tensor_scalar(out=sm[:, :, 0:1], in0=sm[:, :, 0:1], scalar1=0.5, scalar2=None, op0=ALU.mult),
                     reads=[smt], writes=[smt])
                e16, e16t = e16r.next()
                P.op("dve", lambda e16=e16, c8=c8: nc.vector.tensor_tensor(out=e16[:], in0=c8[:, :, 0:16], in1=c8[:, :, 0:1].to_broadcast([128, 8, 16]),
                                                                           op=ALU.subtract), reads=[c8t], writes=[e16t])
                P.op("act", lambda e16=e16: nc.scalar.activation(out=e16[:], in_=e16[:], func=AF.Exp), reads=[e16t], writes=[e16t])
                P.op("dve", lambda sm=sm, e16=e16: nc.vector.tensor_reduce(out=sm[:, :, 1:2], in_=e16[:], op=ALU.add, axis=AX.X), reads=[e16t, smt], writes=[smt])
                P.op("dve", lambda sm=sm: nc.vector.reciprocal(out=sm[:, :, 2:3], in_=sm[:, :, 1:2]), reads=[smt], writes=[smt])
                sv = s[:, :, :].rearrange("p (h c) n -> p h c n", c=2)
                m8v = m8[:, :, :].rearrange("p (h c) k -> p h c k", c=2)
                nA, nAt = nAr.next()
                P.op("dve", lambda nA=nA, sm=sm, sv=sv: nc.vector.tensor_tensor(out=nA[:], in0=sm[:, :, 0:1].to_broadcast([128, 8, 128]), in1=sv[:, :, 0, :],
                                                                                op=ALU.subtract), reads=[smt, st], writes=[nAt])
                E1, E1t = E1r.next()
                E2, E2t = E2r.next()
                t1, t1t = tmr.next()
                P.op("dve", lambda t1=t1, sv=sv, m8v=m8v: nc.vector.tensor_tensor(out=t1[:], in0=sv[:, :, 0, :], in1=m8v[:, :, 0, 0:1].to_broadcast([128, 8, 128]),
                                                                                  op=ALU.subtract), reads=[st, m8t], writes=[t1t])
                P.op("act", lambda t1=t1: nc.scalar.activation(out=t1[:], in_=t1[:], func=AF.Exp), reads=[t1t], writes=[t1t])
                P.op("dve", lambda E1=E1, t1=t1, sm=sm: nc.vector.tensor_tensor(out=E1[:], in0=t1[:], in1=sm[:, :, 2:3].to_broadcast([128, 8, 128]), op=ALU.mult),
                     reads=[t1t, smt], writes=[E1t])
                t2, t2t = tmr.next()
                P.op("dve", lambda t2=t2, sv=sv, m8v=m8v: nc.vector.tensor_tensor(out=t2[:], in0=sv[:, :, 1, :], in1=m8v[:, :, 1, 0:1].to_broadcast([128, 8, 128]),
                                                                                  op=ALU.subtract), reads=[st, m8t], writes=[t2t])
                P.op("act", lambda t2=t2, E2=E2: nc.scalar.activation(out=E2[:], in_=t2[:], func=AF.Exp), reads=[t2t], writes=[E2t])
                for i4 in range(32):
                    pt, ptt = ps.next()
                    for j in range(4):
                        i = i4 * 4 + j
                        mk, mkt = mkr.next()
                        P.op("dve", lambda mk=mk, sv=sv, nA=nA, i=i: nc.vector.tensor_tensor(out=mk[:], in0=sv[:, :, 1, :], in1=nA[:, :, i:i + 1].to_broadcast([128, 8, 128]),
                                                                                             op=ALU.is_ge), reads=[st, nAt], writes=[mkt])
                        P.op("dve", lambda mk=mk, E2=E2: nc.vector.tensor_tensor(out=mk[:], in0=mk[:], in1=E2[:], op=ALU.mult), reads=[mkt, E2t], writes=[mkt])
                        M, Mt = Mr.next()
                        P.op("dve", lambda M=M, mk=mk, E1=E1, i=i: nc.vector.tensor_tensor(out=M[:], in0=mk[:], in1=E1[:, :, i:i + 1].to_broadcast([128, 8, 128]),
                                                                                           op=ALU.mult), reads=[mkt, E1t], writes=[Mt])
                        for h in range(8):
                            P.op("pe", lambda pt=pt, M=M, h=h, j=j: nc.tensor.matmul(pt[:, j * 128:(j + 1) * 128], lhsT=M[:, h, :], rhs=self.ident[:],
                                                                                    start=(h == 0), stop=(h == 7)), reads=[Mt, self.const_t], writes=[ptt])
                    gs, gst = gsr.next()
                    P.op("act", lambda gs=gs, pt=pt: nc.scalar.copy(out=gs[:], in_=pt[:, :].rearrange("p (c t) -> p c t", c=4)), reads=[ptt], writes=[gst])
                    P.op("sp", lambda gs=gs, i4=i4, tl=tl: nc.sync.dma_start(
                        out=gT.ap()[i4 * 4:(i4 + 1) * 4, :, tl * 128:(tl + 1) * 128].rearrange("i j t -> j i t"), in_=gs[:]), reads=[gst], dma=gst)
        P.barrier()

    def phase_peer_main(self, P, ps, hn2T, Ud, Vd, gT, po_d):
        nc = self.nc
        nchunk = 128
        NG = 4
        if self.lim:
            nchunk = 8
        with ExitStack() as es:
            xr = Ring(P, es, "pm_x", [128, 32, 512], BF16, 1)
            accr = Ring(P, es, "pm_acc", [128, 4, D], F32, 1)
            ur = Ring(P, es, "pm_u", [128, D], BF16, 2)
            utr = Ring(P, es, "pm_ut", [128, 32, 128], BF16, 2)
            vr = Ring(P, es, "pm_v", [128, D], BF16, 6)
            gr = Ring(P, es, "pm_g", [128, 512], BF16, 6)
            wr = Ring(P, es, "pm_w", [128, 512], BF16, 6)
            t1r = Ring(P, es, "pm_t1", [128, 512], F32, 2)
            t2r = Ring(P, es, "pm_t2", [128, 512], F32, 2)
            tgs = groups_of(TQ)
            if self.lim:
                tgs = tgs[:1]
            for (t0, n) in tgs:
                ntl = n // 128
                x, xt = xr.next()
                for k0 in (0, 16):
                    P.op("sp", lambda x=x, t0=t0, n=n, k0=k0: nc.sync.dma_start(
                        out=x[:, k0:k0 + 16, :n], in_=hn2T.ap()[k0:k0 + 16, :, t0:t0 + n].rearrange("k p t -> p k t")), writes=[xt], dma=xt)
                acc, acct = accr.next()
                for ip in range(nchunk // NG):
                    Ws = []
                    Vs = []
                    for c in range(NG):
                        i = ip * NG + c
                        u, ut = ur.next()
                        P.op("pool", lambda u=u, i=i: nc.gpsimd.dma_start(out=u[:], in_=Ud.ap()[i * 128:(i + 1) * 128, :], max_dma_last_dim=8192),
                             writes=[ut], dma=ut)
                        v, vt = vr.next()
                        P.op("pool", lambda v=v, i=i: nc.gpsimd.dma_start(out=v[:], in_=Vd.ap()[i * 128:(i + 1) * 128, :], max_dma_last_dim=8192),
                             writes=[vt], dma=vt)
                        g, gt = gr.next()
                        P.op("sp", lambda g=g, i=i, t0=t0, n=n: nc.sync.dma_start(out=g[:, :n], in_=gT.ap()[i, :, t0:t0 + n]), writes=[gt], dma=gt)
                        UT, UTt = utr.next()
                        for kq in range(8):
                            pt, ptt = ps.next()
                            for j in range(4):
                                kc = kq * 4 + j
                                P.op("pe", lambda pt=pt, u=u, kc=kc, j=j: nc.tensor.matmul(pt[:, j * 128:(j + 1) * 128], lhsT=u[:, kc * 128:(kc + 1) * 128],
                                                                                          rhs=self.ident[:], start=True, stop=True), reads=[ut, self.const_t], writes=[ptt])
                            dst = UT[:, kq * 4:(kq + 1) * 4, :]
                            src = pt[:, :].rearrange("p (c t) -> p c t", c=4)
                            P.op("act", lambda dst=dst, src=src: nc.scalar.copy(out=dst, in_=src), reads=[ptt], writes=[UTt])
                        pa, pat = ps.next()
                        for kc in range(32):
                            P.op("pe", lambda pa=pa, UT=UT, x=x, kc=kc, n=n: nc.tensor.matmul(pa[:, :n], lhsT=UT[:, kc, :], rhs=x[:, kc, :n],
                                                                                             start=(kc == 0), stop=(kc == 31)), reads=[UTt, xt], writes=[pat])
                        t1, t1t = t1r.next()
                        t2, t2t = t2r.next()
                        P.op("act", lambda t1=t1, pa=pa, n=n: nc.scalar.activation(out=t1[:, :n], in_=pa[:, :n], func=AF.Square), reads=[pat], writes=[t1t])
                        P.op("act", lambda t1=t1, n=n: nc.scalar.activation(out=t1[:, :n], in_=t1[:, :n], func=AF.Identity, scale=0.044715, bias=self.one_ap),
                             reads=[t1t, self.const_t], writes=[t1t])
                        P.op("dve", lambda t1=t1, t2=t2, pa=pa, n=n: nc.vector.tensor_tensor(out=t2[:, :n], in0=pa[:, :n], in1=t1[:, :n], op=ALU.mult),
                             reads=[pat, t1t], writes=[t2t])
                        P.op("act", lambda t2=t2, n=n: nc.scalar.activation(out=t2[:, :n], in_=t2[:, :n], func=AF.Sigmoid, scale=1.5957691216057308),
                             reads=[t2t], writes=[t2t])
                        P.op("dve", lambda t1=t1, t2=t2, pa=pa, n=n: nc.vector.tensor_tensor(out=t1[:, :n], in0=pa[:, :n], in1=t2[:, :n], op=ALU.mult),
                             reads=[pat, t2t], writes=[t1t])
                        W, Wt = wr.next()
                        P.op("dve", lambda W=W, t1=t1, g=g, n=n: nc.vector.tensor_tensor(out=W[:, :n], in0=t1[:, :n], in1=g[:, :n], op=ALU.mult),
                             reads=[t1t, gt], writes=[Wt])
                        Ws.append((W, Wt))
                        Vs.append((v, vt))
                    for tt in range(ntl):
                        for db in range(8):
                            po, pot = ps.next()
                            for c in range(NG):
                                W, Wt = Ws[c]
                                v, vt = Vs[c]
                                P.op("pe", lambda po=po, W=W, v=v, tt=tt, db=db, c=c: nc.tensor.matmul(
                                    po[:, :], lhsT=W[:, tt * 128:(tt + 1) * 128], rhs=v[:, db * 512:(db + 1) * 512], start=(c == 0), stop=(c == NG - 1)),
                                    reads=[Wt, vt], writes=[pot])
                            dst = acc[:, tt, db * 512:(db + 1) * 512]
                            if ip == 0:
                                P.op("act", lambda dst=dst, po=po: nc.scalar.copy(out=dst, in_=po[:, :]), reads=[pot], writes=[acct])
                            else:
                                P.op("dve", lambda dst=dst, po=po: nc.vector.tensor_tensor(out=dst, in0=po[:, :], in1=dst, op=ALU.add), reads=[pot, acct], writes=[acct])
                for tt in range(ntl):
                    P.op("sp", lambda acc=acc, tt=tt, t0=t0: nc.sync.dma_start(out=po_d.ap()[t0 + tt * 128:t0 + (tt + 1) * 128, :], in_=acc[:, tt, :]),
                         reads=[acct], dma=acct)
        P.barrier()

    def phase_final(self, P, ps, h1, po_d, g_ap, y):
        nc = self.nc
        with ExitStack() as es:
            xin = Ring(P, es, "fi_x", [128, D], F32, 2)
            xad = Ring(P, es, "fi_a", [128, D], F32, 2)
            yr = Ring(P, es, "fi_y", [128, D], F32, 2)
            junk = es.enter_context(nc.sbuf_tensor(self.un("fi_junk"), [128, D], BF16))
            junk_t = P.tr("fi_junk")
            gbc = es.enter_context(nc.sbuf_tensor(self.un("fi_g"), [128, D], F32))
            gbc_t = P.tr("fi_gbc")
            st = Ring(P, es, "fi_st", [128, 4], F32, 4)
            P.op("sp", lambda: nc.sync.dma_start(out=gbc[:], in_=g_ap.partition_broadcast(128)), writes=[gbc_t], dma=gbc_t)
            for i in self.nq_loop():
                x, xt = xin.next()
                a, at = xad.next()
                P.op("sp", lambda x=x, i=i: nc.sync.dma_start(out=x[:], in_=h1.ap()[i * 128:(i + 1) * 128, :]), writes=[xt], dma=xt)
                P.op("sp", lambda a=a, i=i: nc.sync.dma_start(out=a[:], in_=po_d.ap()[i * 128:(i + 1) * 128, :]), writes=[at], dma=at)
                P.op("dve", lambda x=x, a=a: nc.vector.tensor_tensor(out=x[:], in0=x[:], in1=a[:], op=ALU.add), reads=[at, xt], writes=[xt])
                s, s_t = st.next()
                P.op("act", lambda x=x, s=s: nc.scalar.activation(out=junk[:], in_=x[:], func=AF.Square, accum_out=s[:, 0:1]), reads=[xt], writes=[junk_t, s_t])
                P.op("act", lambda s=s: nc.scalar.activation(out=s[:, 1:2], in_=s[:, 0:1], func=AF.Sqrt, scale=1.0 / D, bias=self.eps_ap),
                     reads=[s_t, self.const_t], writes=[s_t])
                P.op("dve", lambda s=s: nc.vector.reciprocal(out=s[:, 2:3], in_=s[:, 1:2]), reads=[s_t], writes=[s_t])
                yt_, ytt = yr.next()
                P.op("dve", lambda x=x, s=s, yt_=yt_: nc.vector.scalar_tensor_tensor(out=yt_[:], in0=x[:], scalar=s[:, 2:3], op0=ALU.mult, in1=gbc[:], op1=ALU.mult),
                     reads=[xt, s_t, gbc_t], writes=[ytt])
                P.op("sp", lambda yt_=yt_, i=i: nc.sync.dma_start(out=y.ap()[i * 128:(i + 1) * 128, :], in_=yt_[:]), reads=[ytt], dma=ytt)
        P.barrier()

    def begin(self, es, consts):
        nc = self.nc
        P = Prog(nc, es)
        self.P = P
        ps = Ring(P, es, "ps", [128, 512], F32, 8, psum=True)
        self.const_t = P.tr("const")
        self.ident = es.enter_context(nc.sbuf_tensor(self.un("ident"), [128, 128], BF16))
        self.ones = es.enter_context(nc.sbuf_tensor(self.un("ones"), [128, 128], BF16))
        self.cst = es.enter_context(nc.sbuf_tensor(self.un("cst"), [128, 64], F32))
        identf = es.enter_context(nc.sbuf_tensor(self.un("identf"), [128, 128], F32))
        self.eps_ap = self.cst[:, 63:64]
        self.one_ap = self.cst[:, 62:63]
        P.op("pool", lambda: nc.gpsimd.memset(identf[:], 1.0), writes=[self.const_t])
        P.op("pool", lambda: nc.gpsimd.affine_select(out=identf[:], in_=identf[:], pattern=[[-1, 128]], compare_op=ALU.is_equal,
                                                    fill=0.0, base=0, channel_multiplier=1), reads=[self.const_t], writes=[self.const_t])
        P.op("pool", lambda: nc.gpsimd.tensor_copy(out=self.ident[:], in_=identf[:]), reads=[self.const_t], writes=[self.const_t])
        P.op("pool", lambda: nc.gpsimd.memset(self.ones[:], 1.0), writes=[self.const_t])
        P.op("sp", lambda: nc.sync.dma_start(out=self.cst[:], in_=consts.ap()), writes=[self.const_t], dma=self.const_t)
        P.barrier()
        return P, ps

    def build(self):
        nc = self.nc
        din, dscr = self.din, self.dscr
        NP = self.nphase
        hall = din("hall", [TK, D])
        hq = din("hq", [TQ, D])
        g_attn = din("g_attn", [D])
        consts = din("consts", [128, 64])
        hnTk = dscr("hnTk", [32, 128, TK])
        hnTq = dscr("hnTq", [32, 128, TQ])
        yout = nc.dram_tensor("y", [TQ, D], F32, kind="ExternalOutput")
        phases = []
        phases.append(lambda P, ps: self.phase_rownorm_T(P, ps, [(hall, hnTk, TK), (hq, hnTq, TQ)], g_attn.ap()))
        if NP > 1:
            wk = din("wk", [D, 1872])
            ropek = din("ropek", [6, 128, TK])
            ckvT = dscr("ckvT", [4, 128, TK]); ckvnT = dscr("ckvnT", [4, 128, TK]); kpeT = dscr("kpeT", [1, 64, TK])
            kbT = dscr("kbT", [4, 128, TK]); kixT = dscr("kixT", [1, 64, TK]); vb = dscr("vb", [TK, 512])
            ku = [dict(kind="plain", c0=i * 128, m=128, dst=ckvT, idx=i) for i in range(4)]
            ku.append(dict(kind="rope", c0=512, m=64, mb=64, dst=kpeT, idx=0, ti=0, rows=[(0, 64)]))
            for g in range(4):
                ku.append(dict(kind="rope", c0=640 + g * 160, m=128, mb=32, dst=kbT, idx=g, ti=1, rows=[(0, 32)]))
            ku.append(dict(kind="rope", c0=1280, m=64, mb=16, dst=kixT, idx=0, ti=2, rows=[(0, 16)]))
            ku.append(dict(kind="tok", c0=1360, m=512, dst=vb, col=0, dtype=BF16))
            phases.append(lambda P, ps: self.phase_gemm(P, ps, wk, D, hnTk, TK, ku, rope=ropek))
            phases.append(lambda P, ps: self.phase_fnorm(P, ps, [(ckvT, ckvnT, 4, TK, self.cst[:, 0:4])]))
        if NP > 3:
            wukv2 = din("wukv2", [512, 4096])
            kanT = dscr("kanT", [16, 128, TK]); va = dscr("va", [TK, 2048])
            u2 = [dict(kind="plain", c0=h * 128, m=128, dst=kanT, idx=h) for h in range(16)]
            u2 += [dict(kind="tok", c0=2048 + j * 512, m=512, dst=va, col=j * 512, dtype=BF16) for j in range(4)]
            phases.append(lambda P, ps: self.phase_gemm(P, ps, wukv2, 512, ckvnT, TK, u2))
        if NP > 4:
            wq = din("wq", [D, 13456])
            ropeq = din("ropeq", [6, 128, TQ])
            cqT = dscr("cqT", [8, 128, TQ]); cqnT = dscr("cqnT", [8, 128, TQ]); qbT = dscr("qbT", [16, 128, TQ]); qixT = dscr("qixT", [8, 128, TQ])
            wix = dscr("wix", [TQ, 16], F32); gaT = dscr("gaT", [32, 128, TQ]); gbT = dscr("gbT", [32, 128, TQ])
            qu = [dict(kind="plain", c0=i * 128, m=128, dst=cqT, idx=i) for i in range(8)]
            qu += [dict(kind="rope", c0=1024 + h * 160, m=128, mb=32, dst=qbT, idx=h, ti=1, rows=[(0, 32)]) for h in range(16)]
            qu += [dict(kind="rope", c0=3584 + b * 208, m=128, mb=80, dst=qixT, idx=b, ti=2, rows=[(0, 16), (64, 16)]) for b in range(8)]
            qu.append(dict(kind="tok", c0=5248, m=16, dst=wix, col=0, dtype=F32))
            qu += [dict(kind="sig", c0=5264 + j * 128, m=128, dst=gaT, idx=j) for j in range(32)]
            qu += [dict(kind="sig", c0=9360 + j * 128, m=128, dst=gbT, idx=j) for j in range(32)]
            phases.append(lambda P, ps: self.phase_gemm(P, ps, wq, D, hnTq, TQ, qu, rope=ropeq))
            phases.append(lambda P, ps: self.phase_fnorm(P, ps, [(cqT, cqnT, 8, TQ, self.cst[:, 4:12])]))
        if NP > 6:
            wuq2 = din("wuq2", [1024, 4096])
            qanT = dscr("qanT", [16, 128, TQ]); qarT = dscr("qarT", [16, 64, TQ])
            u4 = []
            for h in range(16):
                u4.append(dict(kind="plain", c0=h * 256, m=128, dst=qanT, idx=h))
                u4.append(dict(kind="rope", c0=h * 256 + 128, m=64, mb=64, dst=qarT, idx=h, ti=0, rows=[(0, 64)]))
            phases.append(lambda P, ps: self.phase_gemm(P, ps, wuq2, 1024, cqnT, TQ, u4, rope=ropeq))
        if NP > 7:
            cmask = din("cmask", [NQT, 128, 256])
            oT = dscr("oT", [32, 128, TQ])
            phases.append(lambda P, ps: self.phase_mla(P, ps, kanT, kpeT, va, qanT, qarT, cmask, oT, 0))
        if NP > 8:
            phases.append(lambda P, ps: self.phase_dsa(P, ps, kbT, vb, kixT, qbT, qixT, wix, cmask, oT, 16))
        if NP > 9:
            wbr = din("wbr", [D, D])
            mT = dscr("mT", [32, 128, TQ])
            u5 = [dict(kind="branch", c0=j * 128, m=128, dst=mT, idx=j) for j in range(32)]
            phases.append(lambda P, ps: self.phase_gemm(P, ps, wbr, D, oT, TQ, u5, gates=(gaT, gbT)))
        if NP > 10:
            wout = din("wout", [D, D])
            h1 = dscr("h1", [TQ, D], F32)
            u6 = [dict(kind="tok", c0=j * 512, m=512, dst=h1, col=j * 512, dtype=F32, res=hq) for j in range(8)]
            phases.append(lambda P, ps: self.phase_gemm(P, ps, wout, D, mT, TQ, u6))
        if NP > 11:
            g_ffn = din("g_ffn", [D])
            hn2T = dscr("hn2T", [32, 128, TQ])
            phases.append(lambda P, ps: self.phase_rownorm_T(P, ps, [(h1, hn2T, TQ)], g_ffn.ap()))
        if NP > 12:
            wpq = din("wpq", [D, 2048])
            pqT = dscr("pqT", [16, 128, TQ])
            u7 = [dict(kind="plain", c0=j * 128, m=128, dst=pqT, idx=j) for j in range(16)]
            phases.append(lambda P, ps: self.phase_gemm(P, ps, wpq, D, hn2T, TQ, u7))
        if NP > 13:
            subk = din("subk", [128, 16, 128])
            gT = dscr("gT", [128, 128, TQ])
            phases.append(lambda P, ps: self.phase_peer_gates(P, ps, pqT, subk, gT))
        if NP > 14:
            Ud = din("peer_u", [16384, D])
            Vd = din("peer_v", [16384, D])
            po_d = dscr("po", [TQ, D], F32)
            phases.append(lambda P, ps: self.phase_peer_main(P, ps, hn2T, Ud, Vd, gT, po_d))
        if NP > 15:
            g_fin = din("g_fin", [D])
            phases.append(lambda P, ps: self.phase_final(P, ps, h1, po_d, g_fin.ap(), yout))

        with ExitStack() as es:
            P, ps = self.begin(es, consts)
            for f in phases[:NP]:
                f(P, ps)
            P.barrier()
            nw = P.emit()
            self.stats = (len(P.ops), nw)
        return nc


def rope_tab(pos, rot):
    inv = (THETA ** (-np.arange(0, rot, 2, dtype=np.float32) / np.float32(rot))).astype(np.float32)
    ang = pos.astype(np.float32)[:, None] * inv[None, :]
    return np.cos(ang).astype(np.float32), np.sin(ang).astype(np.float32)


def rope_tables(pos):
    T = len(pos)
    out = np.zeros((6, 128, T), np.float32)
    c, s = rope_tab(pos, 64)
    out[0, 0:32] = c.T; out[0, 32:64] = c.T; out[1, 0:32] = -s.T; out[1, 32:64] = s.T
    c, s = rope_tab(pos, 32)
    out[2, 0:16] = c.T; out[2, 16:32] = c.T; out[3, 0:16] = -s.T; out[3, 16:32] = s.T
    c, s = rope_tab(pos, 16)
    for b in (0, 64):
        out[4, b:b + 8] = c.T; out[4, b + 8:b + 16] = c.T; out[5, b:b + 8] = -s.T; out[5, b + 8:b + 16] = s.T
    return out


def swap_halves(w, rot):
    h = rot // 2
    return np.concatenate([w[:, h:rot], w[:, :h]], axis=1)


OFF = dict(cq=0, ckv=1024, kpe=1536, qb=1600, kb=3648, vb=4160, qix=4672, kix=5696, wix=5760, ga=5776, gb=9872)


def core_tiles(hf):
    return [min(2 * p + hf, 32) if (2 * p + hf) <= 32 else 31 for p in range(NQT)]


def prep_inputs(inp, core, names=None):
    b, hf = core // 2, core % 2
    f32 = np.float32
    w_in = inp["w_in"][0]
    need = (lambda k: True) if names is None else (lambda k: k in names)
    out = {}
    hall = np.zeros((TK, D), f32)
    hall[:NMETA] = inp["meta_tokens"]
    hall[NMETA:NMETA + SEQ] = inp["x"][b]
    tiles = core_tiles(hf)
    out["hall"] = hall
    out["hq"] = np.concatenate([hall[t * 128:(t + 1) * 128] for t in tiles], axis=0)
    qpos = np.concatenate([np.arange(t * 128, (t + 1) * 128) for t in tiles])
    out["g_attn"] = np.ascontiguousarray(inp["attn_norm_g"][0])
    consts = np.zeros((128, 64), f32)
    consts[:, 0:4] = inp["kv_norm_g"][0].reshape(4, 128).T
    consts[:, 4:12] = inp["q_norm_g"][0].reshape(8, 128).T
    consts[:, 63] = EPS
    consts[:, 62] = 1.0
    out["consts"] = consts
    if need("wk"):
        cols = [w_in[:, OFF["ckv"]:OFF["ckv"] + 512]]
        kpe = w_in[:, OFF["kpe"]:OFF["kpe"] + 64]
        cols += [kpe, swap_halves(kpe, 64)]
        for g in range(4):
            kb = w_in[:, OFF["kb"] + g * 128:OFF["kb"] + (g + 1) * 128]
            cols += [kb, swap_halves(kb, 32)]
        kix = w_in[:, OFF["kix"]:OFF["kix"] + 64]
        cols += [kix, swap_halves(kix, 16)]
        cols.append(w_in[:, OFF["vb"]:OFF["vb"] + 512])
        out["wk"] = np.ascontiguousarray(np.concatenate(cols, axis=1))
        out["ropek"] = rope_tables(np.arange(TK))
    if need("wukv2"):
        w = inp["w_ukv"][0].reshape(512, 16, 2, 128)
        out["wukv2"] = np.ascontiguousarray(np.concatenate([w[:, :, 0, :].reshape(512, 2048), w[:, :, 1, :].reshape(512, 2048)], axis=1))
    if need("wq"):
        cols = [w_in[:, OFF["cq"]:OFF["cq"] + 1024]]
        for h in range(16):
            qb = w_in[:, OFF["qb"] + h * 128:OFF["qb"] + (h + 1) * 128]
            cols += [qb, swap_halves(qb, 32)]
        z48 = np.zeros((D, 48), f32)
        for bk in range(8):
            q0 = w_in[:, OFF["qix"] + (2 * bk) * 64:OFF["qix"] + (2 * bk + 1) * 64]
            q1 = w_in[:, OFF["qix"] + (2 * bk + 1) * 64:OFF["qix"] + (2 * bk + 2) * 64]
            cols += [q0, q1, swap_halves(q0, 16), z48, swap_halves(q1, 16)]
        cols.append(w_in[:, OFF["wix"]:OFF["wix"] + 16])
        cols.append(w_in[:, OFF["ga"]:OFF["ga"] + 4096])
        cols.append(w_in[:, OFF["gb"]:OFF["gb"] + 4096])
        out["wq"] = np.ascontiguousarray(np.concatenate(cols, axis=1))
        out["ropeq"] = rope_tables(qpos)
    if need("wuq2"):
        w = inp["w_uq"][0]
        cols = []
        for h in range(16):
            rp = w[:, h * 192 + 128:(h + 1) * 192]
            cols += [w[:, h * 192:h * 192 + 128], rp, swap_halves(rp, 64)]
        out["wuq2"] = np.ascontiguousarray(np.concatenate(cols, axis=1))
    if need("cmask"):
        cm = np.zeros((NQT, 128, 256), f32)
        for p, t in enumerate(tiles):
            nk = min(2 * p + 2, NKT)
            qp = t * 128 + np.arange(128)
            kp = (nk - 2) * 128 + np.arange(256)
            cm[p] = np.where(kp[None, :] <= qp[:, None], 0.0, NEG)
        out["cmask"] = cm
    if need("wbr"):
        out["wbr"] = inp["w_branch"][0]
    if need("wout"):
        out["wout"] = inp["w_out"][0]
    if need("g_ffn"):
        out["g_ffn"] = np.ascontiguousarray(inp["ffn_norm_g"][0])
    if need("wpq"):
        out["wpq"] = inp["peer_w_q"][0]
    if need("subk"):
        out["subk"] = np.ascontiguousarray(inp["peer_sub_keys"][0].transpose(3, 0, 1, 2).reshape(128, 16, 128))
    if need("peer_u"):
        out["peer_u"] = inp["peer_u"][0]
        out["peer_v"] = inp["peer_v"][0]
    if need("g_fin"):
        out["g_fin"] = np.ascontiguousarray(inp["final_norm_g"])
    return {k: v for k, v in out.items() if need(k)}


def kernel(**inputs):
    inp = {k: np.asarray(v) for k, v in inputs.items()}
    nphase = int(os.environ.get("MK_NPHASE", "99"))
    debug = tuple(x for x in os.environ.get("MK_DEBUG", "").split(",") if x)
    bld = Builder(nphase, debug)
    nc = bld.build()
    names = set(bld.dram.keys())
    in_maps = [prep_inputs(inp, c, names) for c in range(NCORES)]
    res = run_bass_kernel_spmd(nc, in_maps, core_ids=list(range(NCORES)))
    kernel.last = res
    out = np.zeros((4, SEQ, D), np.float32)
    for c in range(NCORES):
        b, hf = c // 2, c % 2
        y = res.results[c]["y"]
        for p, t in enumerate(core_tiles(hf)):
            if 2 * p + hf > 32:
                continue
            lo, hi = t * 128 - NMETA, (t + 1) * 128 - NMETA
            a, bnd = max(lo, 0), min(hi, SEQ)
            out[b, a:bnd] = y[p * 128 + (a - lo):p * 128 + (bnd - lo)]
    return out
```
